# Optimizing a Trainium2 kernel written in Bass

```python
import jax, jax.numpy as jnp
from jax import lax
import numpy as np

D_MODEL = 2048
BATCH = 4
SEQ = 8192
DEPTH = 1

MIX_WIDTH = D_MODEL
LRU_WIDTH = MIX_WIDTH // 2
GMLP_WIDTH = MIX_WIDTH - LRU_WIDTH
IN_WIDTH = 2 * LRU_WIDTH + 2 * GMLP_WIDTH
LRU_HEADS = 8
LRU_HEAD_DIM = LRU_WIDTH // LRU_HEADS
CONV_WIDTH = 4
CONV_LEFT = 2
LRU_C = 8.0
GMLP_HEADS = 8
GMLP_HEAD_DIM = GMLP_WIDTH // GMLP_HEADS
CHUNK = 128
PEER_HEADS = 8
PEER_KEYS = 128
PEER_EXPERTS = PEER_KEYS * PEER_KEYS
PEER_TOPK = 16
PEER_QDIM = 256
PEER_HALF = PEER_QDIM // 2
PEER_BLOCK = 128
EPS = 1e-6

kernel_name = "hybrid_rglru_spatialgate_peer_encoder"


def rms_norm(x, g):
    xf = x.astype(jnp.float32)
    y = xf * lax.rsqrt(jnp.mean(xf * xf, axis=-1, keepdims=True) + EPS)
    return (y * g.astype(jnp.float32)).astype(x.dtype)


def layer_norm(x, g, b):
    xf = x.astype(jnp.float32)
    mu = jnp.mean(xf, axis=-1, keepdims=True)
    xc = xf - mu
    y = xc * lax.rsqrt(jnp.mean(xc * xc, axis=-1, keepdims=True) + EPS)
    return (y * g.astype(jnp.float32) + b.astype(jnp.float32)).astype(x.dtype)


def centred_depthwise_conv(x, w, b):
    S = x.shape[1]
    xp = jnp.pad(x, ((0, 0), (CONV_LEFT, CONV_WIDTH - 1 - CONV_LEFT), (0, 0)))
    y = b + xp[:, 0:S] * w[0]
    for k in range(1, CONV_WIDTH):
        y = y + xp[:, k:k + S] * w[k]
    return y


def _recurrence_combine(left, right):
    a_l, b_l = left
    a_r, b_r = right
    return a_l * a_r, a_r * b_l + b_r


def rglru(x, w_a, b_a, w_x, b_x, lam, reverse):
    B, S, _ = x.shape
    xh = x.reshape(B, S, LRU_HEADS, LRU_HEAD_DIM)
    r = jax.nn.sigmoid(jnp.einsum('bshc,hce->bshe', xh, w_a.astype(jnp.float32)).reshape(B, S, LRU_WIDTH) + b_a.astype(jnp.float32))
    i = jax.nn.sigmoid(jnp.einsum('bshc,hce->bshe', xh, w_x.astype(jnp.float32)).reshape(B, S, LRU_WIDTH) + b_x.astype(jnp.float32))
    log_a = -LRU_C * jax.nn.softplus(-lam.astype(jnp.float32)) * r
    a = jnp.exp(log_a)
    u = jnp.sqrt(-jnp.expm1(2.0 * log_a)) * (i * x)
    _, h = lax.associative_scan(_recurrence_combine, (a, u), reverse=reverse, axis=1)
    return h


def recurrent_group(xr, gr, conv_w, conv_b, w_a, b_a, w_x, b_x, lam):
    xc = centred_depthwise_conv(xr, conv_w, conv_b).astype(jnp.float32)
    h = (rglru(xc, w_a[0], b_a[0], w_x[0], b_x[0], lam[0], reverse=False)
         + rglru(xc, w_a[1], b_a[1], w_x[1], b_x[1], lam[1], reverse=True))
    return (jax.nn.gelu(gr.astype(jnp.float32)) * h).astype(xr.dtype)


def spatial_gating_group(gu, gv, ln_g, ln_b, w_s, b_s):
    B, S, _ = gv.shape
    u = jax.nn.gelu(gu)
    v = layer_norm(jax.nn.gelu(gv), ln_g, ln_b)
    vc = v.reshape(B, S // CHUNK, CHUNK, GMLP_HEADS, GMLP_HEAD_DIM)
    mixed = jnp.einsum('hpq,bnqhc->bnphc', w_s, vc) + b_s.T[None, None, :, :, None]
    return u * mixed.reshape(B, S, GMLP_WIDTH)


def peer(x, w_q, sub_keys, expert_u, expert_v):
    B, S, D = x.shape
    q = (x @ w_q).reshape(B, S, PEER_HEADS, 2, PEER_HALF)
    s = jnp.einsum('bshpk,hpnk->bshpn', q, sub_keys).astype(jnp.float32)
    top_s, top_i = lax.top_k(s, PEER_TOPK)
    cand_s = (top_s[..., 0, :, None] + top_s[..., 1, None, :]).reshape(B, S, PEER_HEADS, PEER_TOPK * PEER_TOPK)
    cand_i = (top_i[..., 0, :, None] * PEER_KEYS + top_i[..., 1, None, :]).reshape(B, S, PEER_HEADS, PEER_TOPK * PEER_TOPK)
    best_s, pos = lax.top_k(cand_s, PEER_TOPK)
    ids = jnp.take_along_axis(cand_i, pos, axis=-1)
    g = jax.nn.softmax(best_s, axis=-1).astype(x.dtype)
    n_blk = (B * S) // PEER_BLOCK
    xb = x.reshape(n_blk, PEER_BLOCK, D)
    ib = ids.reshape(n_blk, PEER_BLOCK, PEER_HEADS * PEER_TOPK)
    gb = g.reshape(n_blk, PEER_BLOCK, PEER_HEADS * PEER_TOPK)

    def block(args):
        xt, it, gt = args
        u = jnp.take(expert_u, it, axis=0)
        act = jax.nn.gelu(jnp.einsum('td,tkd->tk', xt, u)) * gt
        v = jnp.take(expert_v, it, axis=0)
        return jnp.einsum('tk,tkd->td', act, v)

    out = lax.map(block, (xb, ib, gb))
    return out.reshape(B, S, D)


def setup_inputs(seed: int = 0) -> dict:
    key = jax.random.key(seed)
    ks = jax.random.split(key, 24)
    f32 = jnp.float32
    nrm = lambda k, shape, scale: jax.random.normal(k, shape, f32) * scale
    lam_u = jax.random.uniform(ks[10], (DEPTH, 2, LRU_WIDTH), f32, minval=0.9, maxval=0.999)
    lam_base = lam_u ** (1.0 / LRU_C)
    return {
        "x": nrm(ks[0], (BATCH, SEQ, D_MODEL), 1.0),
        "mix_norm_g": 1.0 + nrm(ks[1], (DEPTH, D_MODEL), 0.02),
        "w_in": nrm(ks[2], (DEPTH, D_MODEL, IN_WIDTH), D_MODEL ** -0.5),
        "conv_w": nrm(ks[3], (DEPTH, CONV_WIDTH, LRU_WIDTH), CONV_WIDTH ** -0.5),
        "conv_b": nrm(ks[4], (DEPTH, LRU_WIDTH), 0.02),
        "lru_w_a": nrm(ks[5], (DEPTH, 2, LRU_HEADS, LRU_HEAD_DIM, LRU_HEAD_DIM), LRU_HEAD_DIM ** -0.5),
        "lru_b_a": nrm(ks[6], (DEPTH, 2, LRU_WIDTH), 0.02),
        "lru_w_x": nrm(ks[7], (DEPTH, 2, LRU_HEADS, LRU_HEAD_DIM, LRU_HEAD_DIM), LRU_HEAD_DIM ** -0.5),
        "lru_b_x": nrm(ks[8], (DEPTH, 2, LRU_WIDTH), 0.02),
        "lru_lambda": jnp.log(lam_base) - jnp.log1p(-lam_base),
        "gmlp_ln_g": 1.0 + nrm(ks[11], (DEPTH, GMLP_WIDTH), 0.02),
        "gmlp_ln_b": nrm(ks[12], (DEPTH, GMLP_WIDTH), 0.02),
        "gmlp_w_s": nrm(ks[13], (DEPTH, GMLP_HEADS, CHUNK, CHUNK), 0.5 * CHUNK ** -0.5),
        "gmlp_b_s": 1.0 + nrm(ks[14], (DEPTH, GMLP_HEADS, CHUNK), 0.1),
        "w_out": nrm(ks[15], (DEPTH, MIX_WIDTH, D_MODEL), MIX_WIDTH ** -0.5),
        "ffn_norm_g": 1.0 + nrm(ks[16], (DEPTH, D_MODEL), 0.02),
        "peer_w_q": nrm(ks[17], (DEPTH, D_MODEL, PEER_HEADS * PEER_QDIM), D_MODEL ** -0.5),
        "peer_sub_keys": nrm(ks[18], (DEPTH, PEER_HEADS, 2, PEER_KEYS, PEER_HALF), PEER_HALF ** -0.5),
        "peer_u": nrm(ks[19], (DEPTH, PEER_EXPERTS, D_MODEL), D_MODEL ** -0.5),
        "peer_v": nrm(ks[20], (DEPTH, PEER_EXPERTS, D_MODEL), PEER_HEADS ** -0.5),
        "final_norm_g": 1.0 + nrm(ks[21], (D_MODEL,), 0.02),
    }


def reference(x, mix_norm_g, w_in, conv_w, conv_b, lru_w_a, lru_b_a, lru_w_x, lru_b_x, lru_lambda,
              gmlp_ln_g, gmlp_ln_b, gmlp_w_s, gmlp_b_s, w_out, ffn_norm_g, peer_w_q, peer_sub_keys,
              peer_u, peer_v, final_norm_g):
    h = x
    for l in range(DEPTH):
        n = rms_norm(h, mix_norm_g[l])
        z = n @ w_in[l]
        xr = z[..., :LRU_WIDTH]
        gr = z[..., LRU_WIDTH:2 * LRU_WIDTH]
        gu = z[..., 2 * LRU_WIDTH:2 * LRU_WIDTH + GMLP_WIDTH]
        gv = z[..., 2 * LRU_WIDTH + GMLP_WIDTH:]
        a_out = recurrent_group(xr, gr, conv_w[l], conv_b[l], lru_w_a[l], lru_b_a[l],
                                lru_w_x[l], lru_b_x[l], lru_lambda[l])
        b_out = spatial_gating_group(gu, gv, gmlp_ln_g[l], gmlp_ln_b[l], gmlp_w_s[l], gmlp_b_s[l])
        h = h + jnp.concatenate([a_out, b_out], axis=-1) @ w_out[l]
        h = h + peer(rms_norm(h, ffn_norm_g[l]), peer_w_q[l], peer_sub_keys[l], peer_u[l], peer_v[l])
    return rms_norm(h, final_norm_g)
```

```python
import contextlib
import numpy as np
import concourse.bass as bass
import concourse.mybir as mybir
from concourse.bass_utils import run_bass_kernel_spmd

F32 = mybir.dt.float32
BF16 = mybir.dt.bfloat16
U32 = mybir.dt.uint32
I32 = mybir.dt.int32
AF = mybir.ActivationFunctionType
ALU = mybir.AluOpType
AX = mybir.AxisListType

D = 2048
T = 4096
NT = T // 128
NE = 16384
EPS = 1e-6
ENGS = ["pe", "act", "dve", "pool", "sp"]


class Buf:
    __slots__ = ("name", "w", "r", "sem", "semv")

    def __init__(self, name):
        self.name = name
        self.w = {}
        self.r = {}
        self.sem = None
        self.semv = 0


class Sched:
    def __init__(self, nc, es):
        self.nc = nc
        self.es = es
        self.ops = {e: [] for e in ENGS}
        self.esem = {e: es.enter_context(nc.semaphore("s_" + e)) for e in ENGS}
        self.seen = {e: {} for e in ENGS}
        self.needed = {e: set() for e in ENGS}
        self.semobj = {}
        self.nsem = 0
        self.all_dma = {}

    def dma_sem(self, buf):
        if buf.sem is None:
            buf.sem = self.es.enter_context(self.nc.semaphore("d%d" % self.nsem))
            self.nsem += 1
            self.semobj[id(buf.sem)] = buf.sem
        return buf.sem

    def _add_waits(self, eng, deps):
        waits = []
        seen = self.seen[eng]
        for key, val in deps.items():
            if key[0] == "e" and key[1] == eng and eng == "pe":
                continue
            if seen.get(key, 0) >= val:
                continue
            seen[key] = val
            waits.append((key, val))
            if key[0] == "e":
                self.needed[key[1]].add(val)
        return waits

    def op(self, eng, name, reads=(), writes=(), dma=None, **kw):
        fn = (name, kw)
        deps = {}
        for b in reads:
            for k, v in b.w.items():
                if deps.get(k, 0) < v:
                    deps[k] = v
        me = ("e", eng)
        for b in writes:
            for d in (b.w, b.r):
                for k, v in d.items():
                    if k == me and dma is None:
                        continue
                    if deps.get(k, 0) < v:
                        deps[k] = v
        lst = self.ops[eng]
        if dma is not None:
            sem = self.dma_sem(dma)
            key = ("d", id(sem))
            if dma.semv > 0:
                deps[key] = max(deps.get(key, 0), dma.semv)
            waits = self._add_waits(eng, deps)
            dma.semv += 16
            val = dma.semv
            lst.append(dict(fn=fn, waits=waits, dma=(sem, 16)))
            self.all_dma[key] = val
        else:
            waits = self._add_waits(eng, deps)
            key = ("e", eng)
            val = len(lst) + 1
            lst.append(dict(fn=fn, waits=waits, dma=None))
        for b in reads:
            if b.r.get(key, 0) < val:
                b.r[key] = val
        for b in writes:
            b.w = {key: val}
            b.r = {}
        return (key, val)

    def wait_all(self, eng):
        deps = dict(self.all_dma)
        for e in ENGS:
            if e != eng and self.ops[e]:
                n = len(self.ops[e])
                while n > 0 and (self.ops[e][n - 1]["dma"] is not None or self.ops[e][n - 1]["fn"] is None):
                    n -= 1
                if n > 0:
                    deps[("e", e)] = n
        waits = self._add_waits(eng, deps)
        self.ops[eng].append(dict(fn=None, waits=waits, dma=None))

    def barrier(self):
        for e in ENGS:
            self.wait_all(e)

    def emit(self):
        nc = self.nc
        valmap = {}
        for e in ENGS:
            idxs = sorted(self.needed[e])
            valmap[e] = {idx: i + 1 for i, idx in enumerate(idxs)}

        def run(ename, eng):
            for i, o in enumerate(self.ops[ename]):
                for key, val in o["waits"]:
                    if key[0] == "e":
                        eng.wait_ge(self.esem[key[1]], valmap[key[1]][val])
                    else:
                        eng.wait_ge(self.semobj[key[1]], val)
                fn = o["fn"]
                if fn is None:
                    continue
                inst = getattr(eng, fn[0])(**fn[1])
                if o["dma"] is not None:
                    inst.then_inc(o["dma"][0], o["dma"][1])
                elif (i + 1) in valmap[ename]:
                    inst.then_inc(self.esem[ename], 1)

        with nc.Block() as block:
            @block.tensor
            def _(eng):
                run("pe", eng)

            @block.scalar
            def _(eng):
                run("act", eng)

            @block.vector
            def _(eng):
                run("dve", eng)

            @block.gpsimd
            def _(eng):
                run("pool", eng)

            @block.sync
            def _(eng):
                run("sp", eng)


class Arena:
    def __init__(self, t, ncols):
        self.t = t
        self.ncols = ncols
        self.off = 0

    def reset(self):
        self.off = 0

    def f32(self, n):
        a = self.t[:, self.off:self.off + n]
        self.off += n
        assert self.off <= self.ncols, ("arena overflow", self.off)
        return a

    def bf16(self, n):
        m = (n + 1) // 2
        a = self.t[:, self.off:self.off + m].bitcast(BF16)
        self.off += m
        assert self.off <= self.ncols, ("arena overflow", self.off)
        return a[:, 0:n]

    def u32(self, n):
        a = self.t[:, self.off:self.off + n].bitcast(U32)
        self.off += n
        assert self.off <= self.ncols
        return a

    def i32(self, n):
        a = self.t[:, self.off:self.off + n].bitcast(I32)
        self.off += n
        assert self.off <= self.ncols
        return a


class Ring:
    def __init__(self, aps, name):
        self.aps = aps
        self.bufs = [Buf("%s%d" % (name, i)) for i in range(len(aps))]
        self.i = 0

    def next(self):
        k = self.i % len(self.aps)
        self.i += 1
        return self.aps[k], self.bufs[k]


ARENA_COLS = 43520
GELU = AF.Gelu_apprx_tanh


def build_nc(stop_after="D", debug=()):
    nc = bass.Bass("TRN2", target_bir_lowering=False)

    def din(name, shape, dt=F32):
        return nc.dram_tensor(name, shape, dt, kind="ExternalInput").ap()

    def dscr(name, shape, dt):
        kind = "ExternalOutput" if name in debug else "Internal"
        return nc.dram_tensor(name, shape, dt, kind=kind).ap()

    x_own = din("x_own", [T, D])
    x_halo = din("x_halo", [128, D])
    x_oth = din("x_oth", [T, D])
    x_ohalo = din("x_ohalo", [128, D])
    masks = din("masks", [128, 2])
    mix_norm_g = din("mix_norm_g", [D])
    w_in = din("w_in", [D, 4096])
    conv_w = din("conv_w", [4, 1024])
    conv_b = din("conv_b", [1024])
    lru_w_a = din("lru_w_a", [2, 8, 128, 128])
    lru_b_a = din("lru_b_a", [2, 1024])
    lru_w_x = din("lru_w_x", [2, 8, 128, 128])
    lru_b_x = din("lru_b_x", [2, 1024])
    lru_lambda = din("lru_lambda", [2, 1024])
    oth_w_a = din("oth_w_a", [8, 128, 128])
    oth_b_a = din("oth_b_a", [1024])
    oth_w_x = din("oth_w_x", [8, 128, 128])
    oth_b_x = din("oth_b_x", [1024])
    oth_lam = din("oth_lam", [1024])
    gmlp_ln_g = din("gmlp_ln_g", [1024])
    gmlp_ln_b = din("gmlp_ln_b", [1024])
    gmlp_w_s = din("gmlp_w_s", [8, 128, 128])
    gmlp_b_s = din("gmlp_b_s", [8, 128])
    w_out = din("w_out", [D, D])
    ffn_norm_g = din("ffn_norm_g", [D])
    peer_w_q = din("peer_w_q", [D, D])
    peer_sub_keys = din("peer_sub_keys", [8, 2, 128, 128])
    need_uv = stop_after in ("C", "D")
    peer_u = din("peer_u", [NE, D]) if need_uv else None
    peer_v = din("peer_v", [NE, D]) if need_uv else None
    final_norm_g = din("final_norm_g", [D])
    out = nc.dram_tensor("out", [T, D], F32, kind="ExternalOutput").ap()

    win_s = dscr("win_s", [128, 16, 4096], BF16)
    xrT_own = dscr("xrT_own", [1024, 4100], F32)
    xrT_oth = dscr("xrT_oth", [1024, 4100], F32)
    ggT = dscr("ggT", [1024, T], F32)
    boutT = dscr("boutT", [1024, T], BF16)
    aoutT = dscr("aoutT", [1024, T], BF16)
    h1s = dscr("h1s", [T, D], F32)
    n2Ts = dscr("n2Ts", [128, 16, T], BF16)
    UTs = dscr("UTs", [128, 128, D], BF16)
    Vs = dscr("Vs", [NE, D], BF16)
    selT = dscr("selT", [3, 128, T], BF16)
    dbg = dscr("dbg", [T, 512], F32)

    es = contextlib.ExitStack()
    with es:
        S = Sched(nc, es)
        arena_t = es.enter_context(nc.sbuf_tensor("arena", [128, ARENA_COLS], F32))
        A = Arena(arena_t, ARENA_COLS)
        consts = es.enter_context(nc.sbuf_tensor("consts", [128, 1024], F32))
        ident_bf = consts[:, 0:64].bitcast(BF16)
        ident_f = consts[:, 64:192]
        iota_bf = consts[:, 192:256].bitcast(BF16)
        g1T = consts[:, 256:272]
        g2T = consts[:, 272:288]
        mk = consts[:, 288:290]
        cint = consts[:, 290:292].bitcast(U32)
        iota_f = consts[:, 320:448]
        pidx_f = consts[:, 448:576]
        iota16 = consts[:, 576:592]
        Bconst = Buf("consts")
        psum = [es.enter_context(nc.psum_tensor("ps%d" % i, [128, 512], F32)) for i in range(8)]
        Bps = [Buf("ps%d" % i) for i in range(8)]

        def psbf(i):
            return psum[i][:, :].bitcast(BF16)

        def psring(idx, bf=False):
            r = Ring([psbf(i) if bf else psum[i] for i in idx], "psr")
            r.bufs = [Bps[i] for i in idx]
            return r

        def dma_in(dst, src, Bdst, reads=(), carrier=None, eng="sp", **kw):
            S.op(eng, "dma_start", reads=list(reads), writes=[Bdst], dma=carrier or Bdst, out=dst, in_=src, **kw)

        S.op("pool", "iota", writes=[Bconst], out=iota_f, pattern=[[1, 128]], base=0, channel_multiplier=0,
             allow_small_or_imprecise_dtypes=True)
        S.op("pool", "iota", writes=[Bconst], out=pidx_f, pattern=[[0, 128]], base=0, channel_multiplier=1,
             allow_small_or_imprecise_dtypes=True)
        S.op("pool", "iota", writes=[Bconst], out=iota16, pattern=[[1, 16]], base=0, channel_multiplier=0,
             allow_small_or_imprecise_dtypes=True)
        S.op("pool", "iota", writes=[Bconst], out=cint[:, 0:1], pattern=[[0, 1]], base=4, channel_multiplier=0)
        S.op("pool", "iota", writes=[Bconst], out=cint[:, 1:2], pattern=[[0, 1]], base=15, channel_multiplier=0)
        S.op("dve", "tensor_tensor", reads=[Bconst], writes=[Bconst], out=ident_f, in0=iota_f, in1=pidx_f, op=ALU.is_equal)
        S.op("dve", "tensor_copy", reads=[Bconst], writes=[Bconst], out=ident_bf, in_=ident_f)
        S.op("dve", "tensor_copy", reads=[Bconst], writes=[Bconst], out=iota_bf, in_=iota_f)
        dma_in(g1T, mix_norm_g.rearrange("(k p) -> p k", p=128), Bconst, carrier=Buf("c1"), allow_slow_non_contiguous=True)
        dma_in(g2T, ffn_norm_g.rearrange("(k p) -> p k", p=128), Bconst, carrier=Buf("c2"), allow_slow_non_contiguous=True)
        dma_in(mk, masks[:, :], Bconst, carrier=Buf("c3"))

        Bwin = Buf("win_s")
        A.reset()
        wt_ring = Ring([A.f32(4096) for _ in range(2)], "wt")
        wb_ring = Ring([A.bf16(4096) for _ in range(2)], "wb")
        for k in range(16):
            wt, Bwt = wt_ring.next()
            wb, Bwb = wb_ring.next()
            dma_in(wt, w_in[k * 128:(k + 1) * 128, :], Bwt)
            S.op("dve", "tensor_scalar", reads=[Bwt, Bconst], writes=[Bwb], out=wb, in0=wt, scalar1=g1T[:, k:k + 1],
                 scalar2=None, op0=ALU.mult)
            S.op("pool", "dma_start", reads=[Bwb], writes=[Bwin], dma=Bwb, out=win_s[:, k, :], in_=wb)

        BUTs = Buf("UTs")
        BVs = Buf("Vs")
        S.barrier()
        if stop_after == "W":
            S.emit()
            return nc

        A.reset()
        wgv = A.bf16(16 * 1024)
        wgv3 = wgv.rearrange("p (k n) -> p k n", k=16)
        lng = A.f32(1024)
        lnb = A.f32(1024)
        bsr = A.f32(1024)
        wsT = A.bf16(1024)
        Bgw = Buf("gmlp_w")
        xt_ring = Ring([A.f32(D) for _ in range(2)], "xt")
        wsraw = xt_ring.aps[0][:, 0:1024]
        Bwsraw = xt_ring.bufs[0]
        dma_in(wgv3, win_s[:, :, 3072:4096], Bgw, reads=[Bwin], carrier=Buf("a1"))
        dma_in(lng, gmlp_ln_g.partition_broadcast(128), Bgw, carrier=Buf("a2"))
        dma_in(lnb, gmlp_ln_b.partition_broadcast(128), Bgw, carrier=Buf("a3"))
        dma_in(bsr, gmlp_b_s.rearrange("h p -> (h p)").partition_broadcast(128), Bgw, carrier=Buf("a4"))
        dma_in(wsraw.rearrange("p (h q) -> p h q", h=8), gmlp_w_s.rearrange("h p q -> p h q"), Bwsraw)
        for h in range(8):
            S.op("pe", "transpose", reads=[Bwsraw, Bconst], writes=[Bps[h // 4]],
                 out=psum[h // 4][:, (h % 4) * 128:(h % 4 + 1) * 128], in_=wsraw[:, h * 128:(h + 1) * 128], identity=ident_f)
        S.op("dve", "tensor_copy", reads=[Bps[0]], writes=[Bgw], out=wsT[:, 0:512], in_=psum[0][:, :])
        S.op("dve", "tensor_copy", reads=[Bps[1]], writes=[Bgw], out=wsT[:, 512:1024], in_=psum[1][:, :])

        xn_ring = Ring([A.bf16(D) for _ in range(4)], "xn")
        nT0 = A.bf16(16 * 512)
        nTs = [nT0, nT0]
        nT3s = [t.rearrange("p (k t) -> p k t", k=16) for t in nTs]
        BnT0 = Buf("nT0")
        BnTs = [BnT0, BnT0]
        wch_ring = Ring([A.bf16(2048) for _ in range(4)], "wch")
        stg_ring = Ring([A.f32(512) for _ in range(3)], "stg")
        guT = A.bf16(8 * 512)
        guT3 = guT.rearrange("p (h t) -> p h t", h=8)
        BguT = Buf("guT")
        gvg_ring = Ring([A.f32(1024) for _ in range(2)], "gvg")
        vtmp = A.f32(1024)
        Bvtmp = Buf("vtmp")
        v_ring = Ring([A.bf16(1024) for _ in range(2)], "v")
        mt_ring = Ring([A.f32(512) for _ in range(2)], "mt")
        bst_ring = Ring([A.bf16(8 * 512) for _ in range(1)], "bst")
        st_ring = Ring([A.f32(16) for _ in range(12)], "st")
        Bxr_own = [Buf("xro%d" % i) for i in range(8)]
        Bxr_oth = [Buf("xrx%d" % i) for i in range(8)]
        Bgg = [Buf("gg%d" % i) for i in range(8)]
        Bbout = Buf("boutT")
        ub_ring = Ring([A.bf16(D) for _ in range(2)], "ub")
        ut_ring = Ring([A.bf16(D) for _ in range(2)], "ut")
        vcar = Ring([None] * 8, "vcar")
        w3_state = [0]
        tp_ring = psring([0, 1], bf=True)
        ip_ring = psring([2, 3])
        gv_ring = psring([4, 5])
        mx_ring = psring([6, 7])

        def w3_chunk(cap=64, store_eng="sp"):
            c = w3_state[0]
            if c >= cap or not need_uv:
                return
            w3_state[0] += 1
            ub, Bub = ub_ring.next()
            ut, But = ut_ring.next()
            dma_in(ub, peer_u[c * 128:(c + 1) * 128, :], Bub, eng="pool")
            for half in range(2):
                tp, Btp = tp_ring.next()
                for kk in range(8):
                    k = half * 8 + kk
                    S.op("pe", "transpose", reads=[Bub, Bconst], writes=[Btp], out=tp[:, kk * 128:(kk + 1) * 128],
                         in_=ub[:, k * 128:(k + 1) * 128], identity=ident_bf)
                if half == 0:
                    S.op("dve", "tensor_copy", reads=[Btp], writes=[But], out=ut[:, 0:1024], in_=tp)
                else:
                    S.op("act", "activation", reads=[Btp], writes=[But], out=ut[:, 1024:2048], in_=tp, func=AF.Copy)
            S.op(store_eng, "dma_start", reads=[But], writes=[], dma=But, out=UTs[c], in_=ut)

        def rms_to_bf16(xt, Bxt, xn, Bxn, st, Bst):
            S.op("act", "activation", reads=[Bxt], writes=[Bxn, Bst], out=xn, in_=xt, func=AF.Square, accum_out=st[:, 0:1])
            S.op("act", "activation", reads=[Bst], writes=[Bst], out=st[:, 1:2], in_=st[:, 0:1], func=AF.Sqrt,
                 scale=1.0 / D, bias=EPS)
            S.op("dve", "reciprocal", reads=[Bst], writes=[Bst], out=st[:, 2:3], in_=st[:, 1:2])
            S.op("act", "activation", reads=[Bxt, Bst], writes=[Bxn], out=xn, in_=xt, func=AF.Copy, scale=st[:, 2:3])

        def transpose16(xn, Bxn, ring, dst3, Bdst, col0):
            for half in range(2):
                tp, Btp = ring.next()
                for kk in range(8):
                    k = half * 8 + kk
                    S.op("pe", "transpose", reads=[Bxn, Bconst], writes=[Btp], out=tp[:, kk * 128:(kk + 1) * 128],
                         in_=xn[:, k * 128:(k + 1) * 128], identity=ident_bf)
                S.op("dve", "tensor_copy", reads=[Btp], writes=[Bdst], out=dst3[:, half * 8:(half + 1) * 8, col0:col0 + 128],
                     in_=tp.rearrange("p (k t) -> p k t", k=8))

        def prep(item):
            outs = []
            for src in item["tiles"]:
                xt, Bxt = xt_ring.next()
                xn, Bxn = xn_ring.next()
                st, Bst = st_ring.next()
                dma_in(xt, src, Bxt, eng="act")
                rms_to_bf16(xt, Bxt, xn, Bxn, st, Bst)
                outs.append((xn, Bxn))
            return outs

        def do_transposes(xns, slot):
            for i, (xn, Bxn) in enumerate(xns):
                transpose16(xn, Bxn, tp_ring, nT3s[slot], BnTs[slot], i * 128)
                w3_chunk()
                w3_chunk()

        def inproj_fm(cc, N, slot):
            wch, Bw = wch_ring.next()
            w3 = wch.rearrange("p (k c) -> p k c", k=16)
            dma_in(w3, win_s[:, :, cc * 128:(cc + 1) * 128], Bw, reads=[Bwin])
            ps, Bp = ip_ring.next()
            for k in range(16):
                S.op("pe", "matmul", reads=[Bw, BnTs[slot]], writes=[Bp], out=ps[:, 0:N], lhsT=w3[:, k, :],
                     rhs=nT3s[slot][:, k, 0:N], start=(k == 0), stop=(k == 15))
            return ps, Bp

        def main(item, slot):
            xrT, Bxr, own, blk = item["xrT"], item["Bxr"], item["own"], item["blk"]
            nT3, BnT = nT3s[slot], BnTs[slot]
            if blk is None:
                for cc in range(8):
                    ps, Bp = inproj_fm(cc, 128, slot)
                    stg, Bs = stg_ring.next()
                    S.op("act", "activation", reads=[Bp], writes=[Bs], out=stg[:, 0:128], in_=ps[:, 0:128], func=AF.Copy)
                    S.op("pool", "dma_start", reads=[Bs], writes=[Bxr[cc]], dma=Bs, out=xrT[cc * 128:(cc + 1) * 128, 0:2],
                         in_=stg[:, 0:2], allow_slow_non_contiguous=True)
                    S.op("pool", "dma_start", reads=[Bs], writes=[Bxr[cc]], dma=Bs, out=xrT[cc * 128:(cc + 1) * 128, 4098:4099],
                         in_=stg[:, 2:3], allow_slow_non_contiguous=True)
                return
            c0 = 2 + blk * 512
            for cc in range(8):
                ps, Bp = inproj_fm(cc, 512, slot)
                stg, Bs = stg_ring.next()
                S.op("act", "activation", reads=[Bp], writes=[Bs], out=stg, in_=ps[:, :], func=AF.Copy)
                S.op("pool", "dma_start", reads=[Bs], writes=[Bxr[cc]], dma=Bs,
                     out=xrT[cc * 128:(cc + 1) * 128, c0:c0 + 512], in_=stg)
            if not own:
                return
            for cc in range(8, 16):
                ps, Bp = inproj_fm(cc, 512, slot)
                stg, Bs = stg_ring.next()
                S.op("act", "activation", reads=[Bp], writes=[Bs], out=stg, in_=ps[:, :], func=GELU)
                S.op("pool", "dma_start", reads=[Bs], writes=[Bgg[cc - 8]], dma=Bs,
                     out=ggT[(cc - 8) * 128:(cc - 7) * 128, blk * 512:(blk + 1) * 512], in_=stg)
            for cc in range(16, 24):
                ps, Bp = inproj_fm(cc, 512, slot)
                S.op("act", "activation", reads=[Bp], writes=[BguT], out=guT3[:, cc - 16, :], in_=ps[:, :], func=GELU)
            bst, Bbst = bst_ring.next()
            bst3 = bst.rearrange("p (h t) -> p h t", h=8)
            vs = {}

            def gv_part(i):
                gvg, Bgvg = gvg_ring.next()
                for nb in range(2):
                    ps, Bp = gv_ring.next()
                    for k in range(16):
                        S.op("pe", "matmul", reads=[BnT, Bgw], writes=[Bp], out=ps[:, :],
                             lhsT=nT3[:, k, i * 128:(i + 1) * 128], rhs=wgv3[:, k, nb * 512:(nb + 1) * 512],
                             start=(k == 0), stop=(k == 15))
                    S.op("act", "activation", reads=[Bp], writes=[Bgvg], out=gvg[:, nb * 512:(nb + 1) * 512],
                         in_=ps[:, :], func=GELU)
                st, Bst = st_ring.next()
                S.op("dve", "bn_stats", reads=[Bgvg], writes=[Bst], out=st[:, 0:6], in_=gvg[:, 0:512])
                S.op("dve", "bn_stats", reads=[Bgvg], writes=[Bst], out=st[:, 6:12], in_=gvg[:, 512:1024])
                S.op("dve", "bn_aggr", reads=[Bst], writes=[Bst], out=st[:, 12:14], in_=st[:, 0:12])
                S.op("act", "activation", reads=[Bst], writes=[Bst], out=st[:, 14:15], in_=st[:, 13:14], func=AF.Sqrt,
                     bias=EPS)
                S.op("dve", "reciprocal", reads=[Bst], writes=[Bst], out=st[:, 15:16], in_=st[:, 14:15])
                S.op("dve", "tensor_scalar", reads=[Bgvg, Bst], writes=[Bvtmp], out=vtmp, in0=gvg, scalar1=st[:, 12:13],
                     scalar2=st[:, 15:16], op0=ALU.subtract, op1=ALU.mult)
                S.op("pool", "tensor_tensor", reads=[Bvtmp, Bgw], writes=[Bvtmp], out=vtmp, in0=vtmp, in1=lng, op=ALU.mult)
                v, Bv = v_ring.next()
                S.op("pool", "tensor_tensor", reads=[Bvtmp, Bgw], writes=[Bv], out=v, in0=vtmp, in1=lnb, op=ALU.add)
                vs[i] = (v, Bv)

            def mix_part(i):
                v, Bv = vs[i]
                for hh in range(2):
                    ps, Bp = mx_ring.next()
                    for h4 in range(4):
                        h = hh * 4 + h4
                        S.op("pe", "matmul", reads=[Bv, Bgw], writes=[Bp], out=ps[:, h4 * 128:(h4 + 1) * 128],
                             lhsT=v[:, h * 128:(h + 1) * 128], rhs=wsT[:, h * 128:(h + 1) * 128], start=True, stop=True)
                    mt, Bmt = mt_ring.next()
                    S.op("dve", "tensor_tensor", reads=[Bp, Bgw], writes=[Bmt], out=mt, in0=ps[:, :],
                         in1=bsr[:, hh * 512:(hh + 1) * 512], op=ALU.add)
                    S.op("dve", "tensor_tensor", reads=[Bmt, BguT], writes=[Bbst],
                         out=bst3[:, hh * 4:(hh + 1) * 4, i * 128:(i + 1) * 128],
                         in0=mt.rearrange("p (h t) -> p h t", h=4),
                         in1=guT3[:, hh * 4:(hh + 1) * 4, i * 128:(i + 1) * 128], op=ALU.mult)

            gv_part(0)
            for i in range(1, 4):
                gv_part(i)
                mix_part(i - 1)
            mix_part(3)
            S.op("pool", "dma_start", reads=[Bbst], writes=[Bbout], dma=Bbst,
                 out=boutT.rearrange("(h c) t -> c h t", h=8)[:, :, blk * 512:(blk + 1) * 512], in_=bst3)

        items = []
        for (x_src, x_hal, xrT, Bxr, own) in ((x_oth, x_ohalo, xrT_oth, Bxr_oth, False), (x_own, x_halo, xrT_own, Bxr_own, True)):
            items.append(dict(tiles=[x_hal[:, :]], xrT=xrT, Bxr=Bxr, own=own, blk=None))
            for blk in range(8):
                items.append(dict(tiles=[x_src[blk * 512 + i * 128:blk * 512 + (i + 1) * 128, :] for i in range(4)],
                                  xrT=xrT, Bxr=Bxr, own=own, blk=blk))
        pre = prep(items[0])
        do_transposes(pre, 0)
        for n, item in enumerate(items):
            nxt = prep(items[n + 1]) if n + 1 < len(items) else None
            main(item, n % 2)
            if nxt is not None:
                do_transposes(nxt, (n + 1) % 2)
        while w3_state[0] < 64 and need_uv:
            w3_chunk()
        S.barrier()
        if stop_after == "A":
            S.emit()
            return nc

        A.reset()
        wg = A.bf16(6 * 8 * 128)
        wg4 = wg.rearrange("p (g h c) -> p g h c", g=6, h=8)
        Bwg = Buf("wg")
        for gi, src in enumerate([lru_w_a[0], lru_w_x[0], lru_w_a[1], lru_w_x[1], oth_w_a, oth_w_x]):
            dma_in(wg4[:, gi], src.rearrange("h ci co -> ci h co"), Bwg, carrier=Buf("bw%d" % gi), eng="pool")
        pp = A.f32(256)
        Bpp = Buf("pp")
        nslow = dict(allow_slow_non_contiguous=True)
        dma_in(pp[:, 0:16].rearrange("p (d h) -> p d h", d=2), lru_b_a.rearrange("d (h p) -> p d h", p=128), Bpp,
               carrier=Buf("bp0"), **nslow)
        dma_in(pp[:, 16:24], oth_b_a.rearrange("(h p) -> p h", p=128), Bpp, carrier=Buf("bp1"), **nslow)
        dma_in(pp[:, 24:40].rearrange("p (d h) -> p d h", d=2), lru_b_x.rearrange("d (h p) -> p d h", p=128), Bpp,
               carrier=Buf("bp2"), **nslow)
        dma_in(pp[:, 40:48], oth_b_x.rearrange("(h p) -> p h", p=128), Bpp, carrier=Buf("bp3"), **nslow)
        dma_in(pp[:, 48:64].rearrange("p (d h) -> p d h", d=2), lru_lambda.rearrange("d (h p) -> p d h", p=128), Bpp,
               carrier=Buf("bp4"), **nslow)
        dma_in(pp[:, 64:72], oth_lam.rearrange("(h p) -> p h", p=128), Bpp, carrier=Buf("bp5"), **nslow)
        dma_in(pp[:, 144:176].rearrange("p (k h) -> p k h", k=4), conv_w.rearrange("k (h p) -> p k h", p=128), Bpp,
               carrier=Buf("bp6"), **nslow)
        dma_in(pp[:, 176:184], conv_b.rearrange("(h p) -> p h", p=128), Bpp, carrier=Buf("bp7"), **nslow)
        S.op("act", "activation", reads=[Bpp], writes=[Bpp], out=pp[:, 120:144], in_=pp[:, 48:72], func=AF.Exp, scale=-1.0)
        S.op("act", "activation", reads=[Bpp], writes=[Bpp], out=pp[:, 120:144], in_=pp[:, 120:144], func=AF.Ln, bias=1.0)
        S.op("dve", "tensor_scalar", reads=[Bpp], writes=[Bpp], out=pp[:, 72:96], in0=pp[:, 120:144], scalar1=-8.0, scalar2=None,
             op0=ALU.mult)
        S.op("dve", "tensor_scalar", reads=[Bpp], writes=[Bpp], out=pp[:, 96:120], in0=pp[:, 120:144], scalar1=-16.0,
             scalar2=None, op0=ALU.mult)
        diag = A.f32(4 * 128)
        Bdiag = Buf("diag")
        XR = A.f32(4100)
        XC = A.f32(T)
        XCb = A.bf16(T)
        rim_aps = [[A.f32(2048) for _ in range(3)] for _ in range(2)]
        rim_bufs = [[Buf("rim%d_%d" % (r, j)) for j in range(3)] for r in range(2)]
        rim_i = [0]
        A1 = A.f32(T)
        U1 = A.f32(T)
        A2 = A.f32(T)
        U2 = A.f32(T)
        OB = XCb
        BXR, BA1, BU1, BA2, BU2, BOB = [Buf(n) for n in "XR A1 U1 A2 U2 OB".split()]
        BXC = [Buf("XC%d" % i) for i in range(8)]
        BXCb = [Buf("XCb%d" % i) for i in range(8)]
        Baout = Buf("aoutT")
        gps = psring([0, 1, 2, 3, 4, 5, 6, 7])

        def conv(hd, xrT, Bxr):
            dma_in(XR[:, 0:4099], xrT[hd * 128:(hd + 1) * 128, 0:4099], BXR, reads=[Bxr[hd]])
            S.op("dve", "tensor_scalar", reads=[BXR, Bpp], writes=BXC, out=XC, in0=XR[:, 0:T],
                 scalar1=pp[:, 144 + hd:145 + hd], scalar2=pp[:, 176 + hd:177 + hd], op0=ALU.mult, op1=ALU.add)
            for k in range(1, 4):
                S.op("dve", "scalar_tensor_tensor", reads=[BXR, Bpp] + BXC, writes=BXC, out=XC, in0=XR[:, k:k + T],
                     scalar=pp[:, 144 + k * 8 + hd:145 + k * 8 + hd], in1=XC, op0=ALU.mult, op1=ALU.add)
            for h2 in range(2):
                S.op("act", "activation", reads=BXC, writes=BXCb[h2 * 4:h2 * 4 + 4], out=XCb[:, h2 * 2048:(h2 + 1) * 2048],
                     in_=XC[:, h2 * 2048:(h2 + 1) * 2048], func=AF.Copy)

        def gates(hd, g, Aa, BAa, Uu, BUu):
            col = g * 8 + hd
            for h2 in range(2):
                r = rim_i[0] % 2
                rim_i[0] += 1
                Rt, It, Mt = rim_aps[r]
                BRt, BIt, BMt = rim_bufs[r]
                for nb in range(4):
                    t0 = h2 * 2048 + nb * 512
                    ps, Bp = gps.next()
                    S.op("pe", "matmul", reads=[Bwg, BXCb[h2 * 4 + nb]], writes=[Bp], out=ps[:, :], lhsT=wg4[:, 2 * g, hd, :],
                         rhs=XCb[:, t0:t0 + 512], start=True, stop=True)
                    S.op("act", "activation", reads=[Bp, Bpp], writes=[BRt], out=Rt[:, nb * 512:(nb + 1) * 512], in_=ps[:, :],
                         func=AF.Sigmoid, bias=pp[:, col:col + 1])
                    ps, Bp = gps.next()
                    S.op("pe", "matmul", reads=[Bwg, BXCb[h2 * 4 + nb]], writes=[Bp], out=ps[:, :], lhsT=wg4[:, 2 * g + 1, hd, :],
                         rhs=XCb[:, t0:t0 + 512], start=True, stop=True)
                    S.op("act", "activation", reads=[Bp, Bpp], writes=[BIt], out=It[:, nb * 512:(nb + 1) * 512], in_=ps[:, :],
                         func=AF.Sigmoid, bias=pp[:, 24 + col:25 + col])
                sl = slice(h2 * 2048, (h2 + 1) * 2048)
                S.op("act", "activation", reads=[BRt, Bpp], writes=[BAa], out=Aa[:, sl], in_=Rt, func=AF.Exp,
                     scale=pp[:, 72 + col:73 + col])
                S.op("pool", "tensor_tensor", reads=[BAa], writes=[BMt], out=Mt, in0=Aa[:, sl], in1=Aa[:, sl], op=ALU.mult)
                S.op("act", "activation", reads=[BMt], writes=[BMt], out=Mt, in_=Mt, func=AF.Sqrt, scale=-1.0, bias=1.0)
                S.op("pool", "tensor_tensor", reads=[BMt, BIt], writes=[BMt], out=Mt, in0=Mt, in1=It, op=ALU.mult)
                S.op("dve", "tensor_tensor", reads=[BMt] + BXC[h2 * 4:h2 * 4 + 4], writes=[BUu], out=Uu[:, sl], in0=Mt,
                     in1=XC[:, sl], op=ALU.mult)

        for hd in range(8):
            if need_uv:
                for c in range(hd * 16, hd * 16 + 16):
                    _, Bvc = vcar.next()
                    S.op("pool", "dma_start", reads=[], writes=[], dma=Bvc, out=Vs[c * 128:(c + 1) * 128, :],
                         in_=peer_v[c * 128:(c + 1) * 128, :])
            conv(hd, xrT_oth, Bxr_oth)
            gates(hd, 2, A1, BA1, U1, BU1)
            S.op("dve", "tensor_tensor_scan", reads=[BA1, BU1], writes=[BA2], out=A2, data0=A1, data1=U1, initial=0.0,
                 op0=ALU.mult, op1=ALU.add)
            S.op("dve", "tensor_tensor_scan", reads=[BA1, BU1], writes=[BU2], out=U2[:, T - 1::-1], data0=A1[:, T - 1::-1],
                 data1=U1[:, T - 1::-1], initial=0.0, op0=ALU.mult, op1=ALU.add)
            S.op("dve", "tensor_tensor", reads=[BA2, Bconst], writes=[Bpp], out=pp[:, 184 + hd:185 + hd], in0=A2[:, T - 1:T],
                 in1=mk[:, 0:1], op=ALU.mult)
            S.op("dve", "tensor_tensor", reads=[BU2, Bconst], writes=[Bpp], out=pp[:, 192 + hd:193 + hd], in0=U2[:, 0:1],
                 in1=mk[:, 1:2], op=ALU.mult)
            conv(hd, xrT_own, Bxr_own)
            gates(hd, 0, A1, BA1, U1, BU1)
            gates(hd, 1, A2, BA2, U2, BU2)
            S.op("dve", "tensor_tensor_scan", reads=[BA1, BU1, Bpp], writes=[BU1], out=U1, data0=A1, data1=U1,
                 initial=pp[:, 184 + hd:185 + hd], op0=ALU.mult, op1=ALU.add)
            S.op("dve", "tensor_tensor_scan", reads=[BA2, BU2, Bpp], writes=[BU2], out=U2[:, T - 1::-1], data0=A2[:, T - 1::-1],
                 data1=U2[:, T - 1::-1], initial=pp[:, 192 + hd:193 + hd], op0=ALU.mult, op1=ALU.add)
            S.op("pool", "tensor_tensor", reads=[BU1, BU2], writes=[BU1], out=U1, in0=U1, in1=U2, op=ALU.add)
            dma_in(XR[:, 0:T], ggT[hd * 128:(hd + 1) * 128, :], BXR, reads=[Bgg[hd]])
            S.op("dve", "tensor_tensor", reads=[BU1, BXR], writes=[BOB] + BXCb, out=OB, in0=U1, in1=XR[:, 0:T], op=ALU.mult)
            S.op("pool", "dma_start", reads=[BOB] + BXCb, writes=[Baout], dma=BOB, out=aoutT[hd * 128:(hd + 1) * 128, :], in_=OB)
        S.barrier()
        if stop_after == "B":
            S.emit()
            return nc

        A.reset()
        wo = A.bf16(16 * D)
        wo3 = wo.rearrange("p (k n) -> p k n", k=16)
        Bwo = Buf("wo")
        dma_in(wo3, w_out.rearrange("(k p) n -> p k n", p=128), Bwo, eng="pool")
        ab_ring = Ring([A.bf16(16 * 128) for _ in range(2)], "ab")
        xt_ring = Ring([A.f32(D) for _ in range(2)], "xt")
        h1_ring = Ring([A.f32(D) for _ in range(2)], "h1")
        xn_ring = Ring([A.bf16(D) for _ in range(3)], "xn")
        n2t_ring = Ring([A.bf16(16 * 128) for _ in range(2)], "n2t")
        st_ring = Ring([A.f32(16) for _ in range(4)], "st")
        g2r = A.f32(D)
        Bg2r = Buf("g2r")
        dma_in(g2r, ffn_norm_g.partition_broadcast(128), Bg2r)
        Bh1s = [Buf("h1s%d" % i) for i in range(NT)]
        Bn2Ts = [Buf("n2Ts%d" % i) for i in range(NT // 2)]
        tp_ring = psring([4, 5, 6, 7], bf=True)
        aoutT3 = aoutT.rearrange("(h c) t -> c h t", h=8)
        boutT3 = boutT.rearrange("(h c) t -> c h t", h=8)
        abcar = [[Buf("abA0"), Buf("abA1")], [Buf("abB0"), Buf("abB1")]]
        pend_c1 = [None]
        ub_ring = Ring([A.bf16(D) for _ in range(2)], "ub2")
        ut_ring = Ring([A.bf16(D) for _ in range(2)], "ut2")
        for i in range(NT):
            ab, Bab = ab_ring.next()
            ab3 = ab.rearrange("p (k t) -> p k t", k=16)
            dma_in(ab3[:, 0:8, :], aoutT3[:, :, i * 128:(i + 1) * 128], Bab, reads=[Baout], carrier=abcar[0][i % 2])
            dma_in(ab3[:, 8:16, :], boutT3[:, :, i * 128:(i + 1) * 128], Bab, reads=[Bbout], carrier=abcar[1][i % 2])
            xt, Bxt = xt_ring.next()
            dma_in(xt, x_own[i * 128:(i + 1) * 128, :], Bxt)
            h1, Bh1 = h1_ring.next()
            for nb in range(4):
                for k in range(16):
                    S.op("pe", "matmul", reads=[Bab, Bwo], writes=[Bps[nb]], out=psum[nb][:, :], lhsT=ab3[:, k, :],
                         rhs=wo3[:, k, nb * 512:(nb + 1) * 512], start=(k == 0), stop=(k == 15))
                S.op("dve", "tensor_tensor", reads=[Bps[nb], Bxt], writes=[Bh1], out=h1[:, nb * 512:(nb + 1) * 512],
                     in0=psum[nb][:, :], in1=xt[:, nb * 512:(nb + 1) * 512], op=ALU.add)
            S.op("pool", "dma_start", reads=[Bh1], writes=[Bh1s[i]], dma=Bh1, out=h1s[i * 128:(i + 1) * 128, :], in_=h1)
            xn, Bxn = xn_ring.next()
            st, Bst = st_ring.next()
            rms_to_bf16(h1, Bh1, xn, Bxn, st, Bst)
            S.op("pool", "tensor_tensor", reads=[Bxn, Bg2r], writes=[Bxn], out=xn, in0=xn, in1=g2r, op=ALU.mult)
            if pend_c1[0] is not None:
                pend_c1[0]()

            def fin(i=i, xn=xn, Bxn=Bxn):
                n2t, Bn2t = n2t_ring.next()
                n2t3 = n2t.rearrange("p (k t) -> p k t", k=16)
                transpose16(xn, Bxn, tp_ring, n2t3, Bn2t, 0)
                S.op("pool", "dma_start", reads=[Bn2t], writes=[Bn2Ts[i // 2]], dma=Bn2t, out=n2Ts[:, :, i * 128:(i + 1) * 128],
                     in_=n2t3)
                w3_chunk(cap=128, store_eng="pool")
                w3_chunk(cap=128, store_eng="pool")
            pend_c1[0] = fin
        pend_c1[0]()
        while w3_state[0] < 128 and need_uv:
            w3_chunk(cap=128, store_eng="pool")
        S.barrier()

        A.reset()
        wq = A.bf16(16 * D)
        wq3 = wq.rearrange("p (k n) -> p k n", k=16)
        Bwq = Buf("wq")
        dma_in(wq3, peer_w_q.rearrange("(k p) n -> p k n", p=128), Bwq, eng="pool")
        kraw = A.f32(16 * 128)
        keysT = A.bf16(16 * 128)
        keysT3 = keysT.rearrange("p (c n) -> p c n", c=16)
        Bkeys = Buf("keys")
        dma_in(kraw.rearrange("p (c k) -> p c k", c=16), peer_sub_keys.rearrange("h p n k -> n (h p) k"), Bkeys,
               carrier=Buf("kr"))
        for cc in range(16):
            S.op("pe", "transpose", reads=[Bkeys, Bconst], writes=[Bps[cc // 4]],
                 out=psum[cc // 4][:, (cc % 4) * 128:(cc % 4 + 1) * 128], in_=kraw[:, cc * 128:(cc + 1) * 128], identity=ident_f)
        for b4 in range(4):
            S.op("dve", "tensor_copy", reads=[Bps[b4]], writes=[Bkeys], out=keysT[:, b4 * 512:(b4 + 1) * 512], in_=psum[b4][:, :])
        n2b_ring = Ring([A.bf16(16 * 512) for _ in range(1)], "n2b")
        qT = A.bf16(16 * 512)
        qT3 = qT.rearrange("p (c t) -> p c t", c=16)
        BqT = Buf("qT")
        sc_ring = Ring([A.f32(D) for _ in range(2)], "sc")
        V01 = A.f32(256)
        I01u = A.u32(256)
        I01f = A.f32(256)
        wk16 = kraw
        cand = A.f32(8 * 256)
        wk2 = A.f32(8 * 256)
        Ex = A.f32(128)
        Cv = A.f32(128)
        Pu = A.u32(128)
        Au = A.u32(128)
        Bu_ = A.u32(128)
        Af = A.f32(128)
        Bf = A.f32(128)
        eqA = A.f32(8 * 256)
        eqB = A.f32(8 * 256)
        sel_ring = Ring([A.f32(3 * 128) for _ in range(2)], "sel")
        selb_ring = Ring([A.bf16(3 * 128) for _ in range(2)], "selb")
        nC0 = A.f32(8)
        Zs = A.f32(8)
        Btk = Buf("topk_tmp")
        BselT = [Buf("selT%d" % i) for i in range(NT // 2)]
        dbgcar = [Buf("dbg0"), Buf("dbg1")]
        qps = psring([4, 5])
        sps = [0, 1, 2, 3]
        tps = psring([6, 7])
        V4 = V01.rearrange("p (s h k) -> p s h k", s=2, h=8)
        I4u = I01u.rearrange("p (s h k) -> p s h k", s=2, h=8)
        I4f = I01f.rearrange("p (s h k) -> p s h k", s=2, h=8)
        cand4 = cand.rearrange("p (h a b) -> p h a b", h=8, a=16)
        eqA4 = eqA.rearrange("p (h k a) -> p h k a", h=8, k=16)
        eqB4 = eqB.rearrange("p (h k a) -> p h k a", h=8, k=16)
        mkb = lambda n: [[Buf("%s%d%d" % (n, h, p)) for p in range(2)] for h in range(8)]
        Bv8, Bi8, Bwk16, Bv8b, Bi8b = mkb("v8"), mkb("i8"), mkb("wk16"), mkb("v8b"), mkb("i8b")
        mk1 = lambda n: [Buf("%s%d" % (n, h)) for h in range(8)]
        Bc8, Bp8, Bwk2, Bc8b, Bp8b = mk1("c8"), mk1("p8"), mk1("wk2"), mk1("c8b"), mk1("p8b")
        Bcand, BI01f, BAu, BBu, BAf, BBf, BEx, BZs, BeqA, BeqB = [Buf(n) for n in "cand I01f Au Bu Af Bf Ex Zs eqA eqB".split()]
        C3 = Cv.rearrange("p (h k) -> p h k", h=8)
        Pu3 = Pu.rearrange("p (h k) -> p h k", h=8)
        for blk in range(8):
            n2b, Bn2b = n2b_ring.next()
            n2b3 = n2b.rearrange("p (k t) -> p k t", k=16)
            dma_in(n2b3, n2Ts[:, :, blk * 512:(blk + 1) * 512], Bn2b, reads=[Bn2Ts[2 * blk], Bn2Ts[2 * blk + 1]])
            for cc in range(16):
                ps, Bp = qps.next()
                for k in range(16):
                    S.op("pe", "matmul", reads=[Bwq, Bn2b], writes=[Bp], out=ps[:, :], lhsT=wq3[:, k, cc * 128:(cc + 1) * 128],
                         rhs=n2b3[:, k, :], start=(k == 0), stop=(k == 15))
                S.op("act", "activation", reads=[Bp], writes=[BqT], out=qT3[:, cc, :], in_=ps[:, :], func=AF.Copy)
            for i in range(4):
                tile = blk * 4 + i
                sc, Bsc = sc_ring.next()
                for cc in range(16):
                    b4 = sps[cc // 4]
                    S.op("pe", "matmul", reads=[BqT, Bkeys], writes=[Bps[b4]], out=psum[b4][:, (cc % 4) * 128:(cc % 4 + 1) * 128],
                         lhsT=qT3[:, cc, i * 128:(i + 1) * 128], rhs=keysT3[:, cc, :], start=True, stop=True)
                for b4 in range(4):
                    S.op("act", "activation", reads=[Bps[sps[b4]]], writes=[Bsc], out=sc[:, b4 * 512:(b4 + 1) * 512],
                         in_=psum[sps[b4]][:, :], func=AF.Copy)
                hp = [(h, p) for h in range(8) for p in range(2)]
                srcs = {(h, p): sc[:, (h * 2 + p) * 128:(h * 2 + p + 1) * 128] for (h, p) in hp}
                for (h, p) in hp:
                    S.op("dve", "max", reads=[Bsc], writes=[Bv8[h][p]], out=V4[:, p, h, 0:8], in_=srcs[(h, p)])
                for (h, p) in hp:
                    S.op("dve", "max_index", reads=[Bsc, Bv8[h][p]], writes=[Bi8[h][p]], out=I4u[:, p, h, 0:8],
                         in_max=V4[:, p, h, 0:8], in_values=srcs[(h, p)])
                for (h, p) in hp:
                    S.op("dve", "match_replace", reads=[Bsc, Bv8[h][p]], writes=[Bwk16[h][p]], out=wk16[:, (h * 2 + p) * 128:(h * 2 + p + 1) * 128],
                         in_to_replace=V4[:, p, h, 0:8], in_values=srcs[(h, p)], imm_value=-1e30)
                for (h, p) in hp:
                    S.op("dve", "max", reads=[Bwk16[h][p]], writes=[Bv8b[h][p]], out=V4[:, p, h, 8:16],
                         in_=wk16[:, (h * 2 + p) * 128:(h * 2 + p + 1) * 128])
                for (h, p) in hp:
                    S.op("dve", "max_index", reads=[Bwk16[h][p], Bv8b[h][p]], writes=[Bi8b[h][p]], out=I4u[:, p, h, 8:16],
                         in_max=V4[:, p, h, 8:16], in_values=wk16[:, (h * 2 + p) * 128:(h * 2 + p + 1) * 128])
                allv = [Bv8[h][p] for (h, p) in hp] + [Bv8b[h][p] for (h, p) in hp]
                alli = [Bi8[h][p] for (h, p) in hp] + [Bi8b[h][p] for (h, p) in hp]
                S.op("dve", "tensor_tensor", reads=allv, writes=[Bcand], out=cand4,
                     in0=V4[:, 0].unsqueeze(3).to_broadcast([128, 8, 16, 16]),
                     in1=V4[:, 1].unsqueeze(2).to_broadcast([128, 8, 16, 16]), op=ALU.add)
                S.op("dve", "tensor_copy", reads=alli, writes=[BI01f], out=I01f, in_=I01u)
                for h in range(8):
                    S.op("dve", "max", reads=[Bcand], writes=[Bc8[h]], out=C3[:, h, 0:8], in_=cand[:, h * 256:(h + 1) * 256])
                for h in range(8):
                    S.op("dve", "max_index", reads=[Bcand, Bc8[h]], writes=[Bp8[h]], out=Pu3[:, h, 0:8], in_max=C3[:, h, 0:8],
                         in_values=cand[:, h * 256:(h + 1) * 256])
                for h in range(8):
                    S.op("dve", "match_replace", reads=[Bcand, Bc8[h]], writes=[Bwk2[h]], out=wk2[:, h * 256:(h + 1) * 256],
                         in_to_replace=C3[:, h, 0:8], in_values=cand[:, h * 256:(h + 1) * 256], imm_value=-1e30)
                for h in range(8):
                    S.op("dve", "max", reads=[Bwk2[h]], writes=[Bc8b[h]], out=C3[:, h, 8:16], in_=wk2[:, h * 256:(h + 1) * 256])
                for h in range(8):
                    S.op("dve", "max_index", reads=[Bwk2[h], Bc8b[h]], writes=[Bp8b[h]], out=Pu3[:, h, 8:16], in_max=C3[:, h, 8:16],
                         in_values=wk2[:, h * 256:(h + 1) * 256])
                allc = Bc8 + Bc8b
                allp = Bp8 + Bp8b
                S.op("dve", "tensor_tensor", reads=allp + [Bconst], writes=[BAu], out=Au, in0=Pu,
                     in1=cint[:, 0:1].to_broadcast([128, 128]), op=ALU.logical_shift_right)
                S.op("dve", "tensor_tensor", reads=allp + [Bconst], writes=[BBu], out=Bu_, in0=Pu,
                     in1=cint[:, 1:2].to_broadcast([128, 128]), op=ALU.bitwise_and)
                sel, Bsel = sel_ring.next()
                sel4 = sel.rearrange("p (s h k) -> p s h k", s=3, h=8)
                S.op("dve", "tensor_tensor", reads=allc, writes=[BEx], out=Ex.rearrange("p (h k) -> p h k", h=8), in0=C3,
                     in1=C3[:, :, 0:1].to_broadcast([128, 8, 16]), op=ALU.subtract)
                S.op("dve", "tensor_copy", reads=[BAu], writes=[BAf], out=Af, in_=Au)
                S.op("dve", "tensor_copy", reads=[BBu], writes=[BBf], out=Bf, in_=Bu_)
                S.op("act", "activation", reads=[BEx], writes=[BEx], out=Ex, in_=Ex, func=AF.Exp)
                for XF, BXF, e4, Be, s_ in ((Af, BAf, eqA4, BeqA, 0), (Bf, BBf, eqB4, BeqB, 1)):
                    S.op("dve", "tensor_tensor", reads=[BXF, Bconst], writes=[Be], out=e4,
                         in0=XF.rearrange("p (h k) -> p h k", h=8).unsqueeze(3).to_broadcast([128, 8, 16, 16]),
                         in1=iota16.unsqueeze(1).unsqueeze(1).to_broadcast([128, 8, 16, 16]), op=ALU.is_equal)
                for XF, BXF, e4, Be, s_ in ((Af, BAf, eqA4, BeqA, 0), (Bf, BBf, eqB4, BeqB, 1)):
                    S.op("dve", "tensor_tensor", reads=[Be, BI01f], writes=[Be], out=e4, in0=e4,
                         in1=I4f[:, s_].unsqueeze(2).to_broadcast([128, 8, 16, 16]), op=ALU.mult)
                S.op("dve", "tensor_reduce", reads=[BEx], writes=[BZs], out=Zs, in_=Ex.rearrange("p (h k) -> p h k", h=8),
                     axis=AX.X, op=ALU.add)
                S.op("dve", "reciprocal", reads=[BZs], writes=[BZs], out=Zs, in_=Zs)
                S.op("dve", "tensor_reduce", reads=[BeqA], writes=[Bsel], out=sel4[:, 0], in_=eqA4, axis=AX.X, op=ALU.add)
                S.op("dve", "tensor_reduce", reads=[BeqB], writes=[Bsel], out=sel4[:, 1], in_=eqB4, axis=AX.X, op=ALU.add)
                S.op("dve", "tensor_tensor", reads=[BEx, BZs], writes=[Bsel], out=sel4[:, 2], in0=Ex.rearrange("p (h k) -> p h k", h=8),
                     in1=Zs.unsqueeze(2).to_broadcast([128, 8, 16]), op=ALU.mult)
                if "dbg" in debug:
                    S.op("pool", "dma_start", reads=[Bsel], writes=[], dma=dbgcar[tile % 2],
                         out=dbg[tile * 128:(tile + 1) * 128, 0:384], in_=sel)
                selb, Bselb = selb_ring.next()
                ps, Bp = tps.next()
                for s_ in range(3):
                    S.op("pe", "transpose", reads=[Bsel, Bconst], writes=[Bp], out=ps[:, s_ * 128:(s_ + 1) * 128],
                         in_=sel[:, s_ * 128:(s_ + 1) * 128], identity=ident_f)
                S.op("act", "activation", reads=[Bp], writes=[Bselb], out=selb, in_=ps[:, 0:384], func=AF.Copy)
                S.op("pool", "dma_start", reads=[Bselb], writes=[BselT[tile // 2]], dma=Bselb,
                     out=selT.rearrange("s p t -> p s t")[:, :, tile * 128:(tile + 1) * 128],
                     in_=selb.rearrange("p (s t) -> p s t", s=3))
        S.barrier()
        if stop_after == "C":
            S.emit()
            return nc

        A.reset()
        TB = 256
        G = A.bf16(128 * TB)
        G3 = G.rearrange("p (c t) -> p c t", c=128)
        BG = Buf("G")
        fg = A.f32(D)
        Bfg = Buf("fg")
        dma_in(fg, final_norm_g.partition_broadcast(128), Bfg)
        tab_ring = Ring([A.bf16(3 * TB) for _ in range(2)], "tab")
        n2d_ring = Ring([A.bf16(16 * TB) for _ in range(2)], "n2d")
        pq_aps = [A.bf16(2 * 16 * 128) for _ in range(2)]
        pq_bufs = [[(Buf("p%d_%d" % (r, t)), Buf("q%d_%d" % (r, t))) for t in range(16)] for r in range(2)]
        ut_ring = Ring([A.bf16(D) for _ in range(6)], "utd")
        vt_ring = Ring([A.bf16(1024) for _ in range(8)], "vtd")
        gt_ring = Ring([A.bf16(TB) for _ in range(2)], "gt")
        hf_ring = Ring([A.f32(D) for _ in range(2)], "hf")
        sqj = A.bf16(D)
        Bsqj = Buf("sqj")
        st_ring = Ring([A.f32(16) for _ in range(4)], "st")
        gps2 = psring([0, 1])
        xps = psring([2, 3])
        Bout = Buf("out")
        for tb in range(T // TB):
            t0 = tb * TB
            tab, Btab = tab_ring.next()
            tab3 = tab.rearrange("p (s t) -> p s t", s=3)
            dma_in(tab3, selT.rearrange("s p t -> p s t")[:, :, t0:t0 + TB], Btab, reads=[BselT[tb]])
            n2d, Bn2d = n2d_ring.next()
            n2d3 = n2d.rearrange("p (k t) -> p k t", k=16)
            dma_in(n2d3, n2Ts[:, :, t0:t0 + TB], Bn2d, reads=[Bn2Ts[tb]])
            for sb in range(TB // 16):
                pq = pq_aps[sb % 2]
                pq4 = pq.rearrange("p (s t i) -> p s t i", s=2, t=16)
                for tt in range(16):
                    t = sb * 16 + tt
                    BP_, BQ_ = pq_bufs[sb % 2][tt]
                    S.op("dve", "tensor_scalar", reads=[Btab, Bconst], writes=[BP_], out=pq4[:, 0, tt, :], in0=iota_bf,
                         scalar1=tab3[:, 0, t:t + 1], scalar2=None, op0=ALU.is_equal)
                    S.op("dve", "tensor_scalar", reads=[Btab, Bconst], writes=[BQ_], out=pq4[:, 1, tt, :], in0=iota_bf,
                         scalar1=tab3[:, 1, t:t + 1], scalar2=tab3[:, 2, t:t + 1], op0=ALU.is_equal, op1=ALU.mult)
                for q4 in range(4):
                    ps, Bp = gps2.next()
                    for t4 in range(4):
                        tt = q4 * 4 + t4
                        S.op("pe", "matmul", reads=list(pq_bufs[sb % 2][tt]), writes=[Bp], out=ps[:, t4 * 128:(t4 + 1) * 128], lhsT=pq4[:, 1, tt, :],
                             rhs=pq4[:, 0, tt, :], start=True, stop=True)
                    tg = sb * 16 + q4 * 4
                    S.op("act", "activation", reads=[Bp], writes=[BG], out=G3[:, :, tg:tg + 4],
                         in_=ps.rearrange("p (t i) -> p i t", t=4), func=AF.Copy)
            for c in range(128):
                ut, But = ut_ring.next()
                ut3 = ut.rearrange("p (k e) -> p k e", k=16)
                dma_in(ut, UTs[c], But, reads=[BUTs])
                ps, Bp = xps.next()
                for k in range(16):
                    S.op("pe", "matmul", reads=[But, Bn2d], writes=[Bp], out=ps[:, 0:TB], lhsT=ut3[:, k, :], rhs=n2d3[:, k, :],
                         start=(k == 0), stop=(k == 15))
                gt, Bgt = gt_ring.next()
                S.op("act", "activation", reads=[Bp], writes=[Bgt], out=gt, in_=ps[:, 0:TB], func=GELU)
                S.op("dve", "tensor_tensor", reads=[Bgt, BG], writes=[BG], out=G3[:, c, :], in0=gt, in1=G3[:, c, :], op=ALU.mult)
            hfs = []
            for tl in range(2):
                hf, Bhf = hf_ring.next()
                dma_in(hf, h1s[t0 + tl * 128:t0 + (tl + 1) * 128, :], Bhf, reads=[Bh1s[tb * 2 + tl]])
                hfs.append((hf, Bhf))
            for dh in range(2):
                for c in range(128):
                    vt, Bvt = vt_ring.next()
                    dma_in(vt, Vs[c * 128:(c + 1) * 128, dh * 1024:(dh + 1) * 1024], Bvt, reads=[BVs])
                    for tl in range(2):
                        for nb in range(2):
                            b = 4 + tl * 2 + nb
                            S.op("pe", "matmul", reads=[BG, Bvt], writes=[Bps[b]], out=psum[b][:, :],
                                 lhsT=G3[:, c, tl * 128:(tl + 1) * 128], rhs=vt[:, nb * 512:(nb + 1) * 512],
                                 start=(c == 0), stop=(c == 127))
                for tl in range(2):
                    hf, Bhf = hfs[tl]
                    for nb in range(2):
                        b = 4 + tl * 2 + nb
                        sl = slice(dh * 1024 + nb * 512, dh * 1024 + (nb + 1) * 512)
                        S.op("dve", "tensor_tensor", reads=[Bps[b], Bhf], writes=[Bhf], out=hf[:, sl], in0=psum[b][:, :],
                             in1=hf[:, sl], op=ALU.add)
            for tl in range(2):
                hf, Bhf = hfs[tl]
                ot, Bot = hf, Bhf
                st, Bst = st_ring.next()
                S.op("act", "activation", reads=[Bhf], writes=[Bsqj, Bst], out=sqj, in_=hf, func=AF.Square, accum_out=st[:, 0:1])
                S.op("act", "activation", reads=[Bst], writes=[Bst], out=st[:, 1:2], in_=st[:, 0:1], func=AF.Sqrt, scale=1.0 / D,
                     bias=EPS)
                S.op("dve", "reciprocal", reads=[Bst], writes=[Bst], out=st[:, 2:3], in_=st[:, 1:2])
                S.op("act", "activation", reads=[Bhf, Bst], writes=[Bot], out=ot, in_=hf, func=AF.Copy, scale=st[:, 2:3])
                S.op("pool", "tensor_tensor", reads=[Bot, Bfg], writes=[Bot], out=ot, in0=ot, in1=fg, op=ALU.mult)
                S.op("pool", "dma_start", reads=[Bot], writes=[Bout], dma=Bot, out=out[t0 + tl * 128:t0 + (tl + 1) * 128, :], in_=ot)
        S.barrier()
        S.emit()
    return nc


def make_in_maps(inputs):
    x = np.asarray(inputs["x"], dtype=np.float32)
    sq = lambda k: np.ascontiguousarray(np.asarray(inputs[k], dtype=np.float32)[0])
    shared = {
        "mix_norm_g": sq("mix_norm_g"), "w_in": sq("w_in"), "conv_w": sq("conv_w"), "conv_b": sq("conv_b"),
        "lru_w_a": sq("lru_w_a"), "lru_b_a": sq("lru_b_a"), "lru_w_x": sq("lru_w_x"), "lru_b_x": sq("lru_b_x"),
        "lru_lambda": sq("lru_lambda"), "gmlp_ln_g": sq("gmlp_ln_g"), "gmlp_ln_b": sq("gmlp_ln_b"),
        "gmlp_w_s": sq("gmlp_w_s"), "gmlp_b_s": sq("gmlp_b_s"), "w_out": sq("w_out"), "ffn_norm_g": sq("ffn_norm_g"),
        "peer_w_q": sq("peer_w_q"), "peer_sub_keys": sq("peer_sub_keys"), "peer_u": sq("peer_u"), "peer_v": sq("peer_v"),
        "final_norm_g": np.ascontiguousarray(np.asarray(inputs["final_norm_g"], dtype=np.float32)),
    }
    in_maps = []
    for c in range(8):
        b, hf = c // 2, c % 2
        s0 = hf * T
        o0 = (1 - hf) * T

        def halo(st):
            h = np.zeros((128, D), np.float32)
            if st >= 2:
                h[0:2] = x[b, st - 2:st]
            if st + T < 2 * T:
                h[2] = x[b, st + T]
            return h
        d_o = 0 if hf == 1 else 1
        m = np.zeros((128, 2), np.float32)
        m[:, 0] = 1.0 if hf == 1 else 0.0
        m[:, 1] = 1.0 if hf == 0 else 0.0
        d = dict(shared)
        d.update({
            "x_own": np.ascontiguousarray(x[b, s0:s0 + T]), "x_halo": halo(s0),
            "x_oth": np.ascontiguousarray(x[b, o0:o0 + T]), "x_ohalo": halo(o0), "masks": m,
            "oth_w_a": np.ascontiguousarray(shared["lru_w_a"][d_o]), "oth_b_a": np.ascontiguousarray(shared["lru_b_a"][d_o]),
            "oth_w_x": np.ascontiguousarray(shared["lru_w_x"][d_o]), "oth_b_x": np.ascontiguousarray(shared["lru_b_x"][d_o]),
            "oth_lam": np.ascontiguousarray(shared["lru_lambda"][d_o]),
        })
        in_maps.append(d)
    return in_maps


_NC_CACHE = {}


def kernel(**inputs):
    if "nc" not in _NC_CACHE:
        _NC_CACHE["nc"] = build_nc()
    nc = _NC_CACHE["nc"]
    in_maps = make_in_maps(inputs)
    res = run_bass_kernel_spmd(nc, in_maps, core_ids=list(range(8)))
    outs = [np.asarray(res.results[c]["out"], dtype=np.float32) for c in range(8)]
    return np.stack(outs, 0).reshape(4, 2 * T, D)
```

```python
import contextlib
import numpy as np
import concourse.bass as bass
import concourse.mybir as mybir
from concourse.bass_utils import run_bass_kernel_spmd

F32 = mybir.dt.float32
BF16 = mybir.dt.bfloat16
U32 = mybir.dt.uint32
I32 = mybir.dt.int32
AF = mybir.ActivationFunctionType
ALU = mybir.AluOpType
AX = mybir.AxisListType

D = 2048
T = 4096
NT = T // 128
NE = 16384
EPS = 1e-6
ENGS = ["pe", "act", "dve", "pool", "sp"]


class Buf:
    __slots__ = ("name", "w", "r", "sem", "semv")

    def __init__(self, name):
        self.name = name
        self.w = {}
        self.r = {}
        self.sem = None
        self.semv = 0


class Sched:
    def __init__(self, nc, es):
        self.nc = nc
        self.es = es
        self.ops = {e: [] for e in ENGS}
        self.esem = {e: es.enter_context(nc.semaphore("s_" + e)) for e in ENGS}
        self.seen = {e: {} for e in ENGS}
        self.needed = {e: set() for e in ENGS}
        self.semobj = {}
        self.nsem = 0
        self.all_dma = {}

    def dma_sem(self, buf):
        if buf.sem is None:
            buf.sem = self.es.enter_context(self.nc.semaphore("d%d" % self.nsem))
            self.nsem += 1
            self.semobj[id(buf.sem)] = buf.sem
        return buf.sem

    def _add_waits(self, eng, deps):
        waits = []
        seen = self.seen[eng]
        for key, val in deps.items():
            if key[0] == "e" and key[1] == eng and eng == "pe":
                continue
            if seen.get(key, 0) >= val:
                continue
            seen[key] = val
            waits.append((key, val))
            if key[0] == "e":
                self.needed[key[1]].add(val)
        return waits

    def op(self, eng, name, reads=(), writes=(), dma=None, **kw):
        fn = (name, kw)
        deps = {}
        for b in reads:
            for k, v in b.w.items():
                if deps.get(k, 0) < v:
                    deps[k] = v
        me = ("e", eng)
        for b in writes:
            for d in (b.w, b.r):
                for k, v in d.items():
                    if k == me and dma is None:
                        continue
                    if deps.get(k, 0) < v:
                        deps[k] = v
        lst = self.ops[eng]
        if dma is not None:
            sem = self.dma_sem(dma)
            key = ("d", id(sem))
            if dma.semv > 0:
                deps[key] = max(deps.get(key, 0), dma.semv)
            waits = self._add_waits(eng, deps)
            dma.semv += 16
            val = dma.semv
            lst.append(dict(fn=fn, waits=waits, dma=(sem, 16)))
            self.all_dma[key] = val
        else:
            waits = self._add_waits(eng, deps)
            key = ("e", eng)
            val = len(lst) + 1
            lst.append(dict(fn=fn, waits=waits, dma=None))
        for b in reads:
            if b.r.get(key, 0) < val:
                b.r[key] = val
        for b in writes:
            b.w = {key: val}
            b.r = {}
        return (key, val)

    def wait_all(self, eng):
        deps = dict(self.all_dma)
        for e in ENGS:
            if e != eng and self.ops[e]:
                n = len(self.ops[e])
                while n > 0 and (self.ops[e][n - 1]["dma"] is not None or self.ops[e][n - 1]["fn"] is None):
                    n -= 1
                if n > 0:
                    deps[("e", e)] = n
        waits = self._add_waits(eng, deps)
        self.ops[eng].append(dict(fn=None, waits=waits, dma=None))

    def barrier(self):
        for e in ENGS:
            self.wait_all(e)

    def emit(self):
        nc = self.nc
        valmap = {}
        for e in ENGS:
            idxs = sorted(self.needed[e])
            valmap[e] = {idx: i + 1 for i, idx in enumerate(idxs)}

        def run(ename, eng):
            for i, o in enumerate(self.ops[ename]):
                for key, val in o["waits"]:
                    if key[0] == "e":
                        eng.wait_ge(self.esem[key[1]], valmap[key[1]][val])
                    else:
                        eng.wait_ge(self.semobj[key[1]], val)
                fn = o["fn"]
                if fn is None:
                    continue
                inst = getattr(eng, fn[0])(**fn[1])
                if o["dma"] is not None:
                    inst.then_inc(o["dma"][0], o["dma"][1])
                elif (i + 1) in valmap[ename]:
                    inst.then_inc(self.esem[ename], 1)

        with nc.Block() as block:
            @block.tensor
            def _(eng):
                run("pe", eng)

            @block.scalar
            def _(eng):
                run("act", eng)

            @block.vector
            def _(eng):
                run("dve", eng)

            @block.gpsimd
            def _(eng):
                run("pool", eng)

            @block.sync
            def _(eng):
                run("sp", eng)


class Arena:
    def __init__(self, t, ncols):
        self.t = t
        self.ncols = ncols
        self.off = 0

    def reset(self):
        self.off = 0

    def f32(self, n):
        a = self.t[:, self.off:self.off + n]
        self.off += n
        assert self.off <= self.ncols, ("arena overflow", self.off)
        return a

    def bf16(self, n):
        m = (n + 1) // 2
        a = self.t[:, self.off:self.off + m].bitcast(BF16)
        self.off += m
        assert self.off <= self.ncols, ("arena overflow", self.off)
        return a[:, 0:n]

    def u32(self, n):
        a = self.t[:, self.off:self.off + n].bitcast(U32)
        self.off += n
        assert self.off <= self.ncols
        return a

    def i32(self, n):
        a = self.t[:, self.off:self.off + n].bitcast(I32)
        self.off += n
        assert self.off <= self.ncols
        return a


class Ring:
    def __init__(self, aps, name):
        self.aps = aps
        self.bufs = [Buf("%s%d" % (name, i)) for i in range(len(aps))]
        self.i = 0

    def next(self):
        k = self.i % len(self.aps)
        self.i += 1
        return self.aps[k], self.bufs[k]


ARENA_COLS = 43520
GELU = AF.Gelu_apprx_tanh


def build_nc(stop_after="D", debug=()):
    nc = bass.Bass("TRN2", target_bir_lowering=False)

    def din(name, shape, dt=F32):
        return nc.dram_tensor(name, shape, dt, kind="ExternalInput").ap()

    def dscr(name, shape, dt):
        kind = "ExternalOutput" if name in debug else "Internal"
        return nc.dram_tensor(name, shape, dt, kind=kind).ap()

    x_own = din("x_own", [T, D])
    x_halo = din("x_halo", [128, D])
    x_oth = din("x_oth", [T, D])
    x_ohalo = din("x_ohalo", [128, D])
    masks = din("masks", [128, 2])
    mix_norm_g = din("mix_norm_g", [D])
    w_in = din("w_in", [D, 4096])
    conv_w = din("conv_w", [4, 1024])
    conv_b = din("conv_b", [1024])
    lru_w_a = din("lru_w_a", [2, 8, 128, 128])
    lru_b_a = din("lru_b_a", [2, 1024])
    lru_w_x = din("lru_w_x", [2, 8, 128, 128])
    lru_b_x = din("lru_b_x", [2, 1024])
    lru_lambda = din("lru_lambda", [2, 1024])
    oth_w_a = din("oth_w_a", [8, 128, 128])
    oth_b_a = din("oth_b_a", [1024])
    oth_w_x = din("oth_w_x", [8, 128, 128])
    oth_b_x = din("oth_b_x", [1024])
    oth_lam = din("oth_lam", [1024])
    gmlp_ln_g = din("gmlp_ln_g", [1024])
    gmlp_ln_b = din("gmlp_ln_b", [1024])
    gmlp_w_s = din("gmlp_w_s", [8, 128, 128])
    gmlp_b_s = din("gmlp_b_s", [8, 128])
    w_out = din("w_out", [D, D])
    ffn_norm_g = din("ffn_norm_g", [D])
    peer_w_q = din("peer_w_q", [D, D])
    peer_sub_keys = din("peer_sub_keys", [8, 2, 128, 128])
    need_uv = stop_after in ("C", "D")
    peer_u = din("peer_u", [NE, D]) if need_uv else None
    peer_v = din("peer_v", [NE, D]) if need_uv else None
    final_norm_g = din("final_norm_g", [D])
    out = nc.dram_tensor("out", [T, D], F32, kind="ExternalOutput").ap()

    win_s = dscr("win_s", [128, 16, 4096], BF16)
    xrT_own = dscr("xrT_own", [1024, 4100], F32)
    xrT_oth = dscr("xrT_oth", [1024, 4100], F32)
    ggT = dscr("ggT", [1024, T], F32)
    boutT = dscr("boutT", [1024, T], BF16)
    aoutT = dscr("aoutT", [1024, T], BF16)
    h1s = dscr("h1s", [T, D], F32)
    n2Ts = dscr("n2Ts", [128, 16, T], BF16)
    UTs = dscr("UTs", [128, 128, D], BF16)
    Vs = dscr("Vs", [NE, D], BF16)
    selT = dscr("selT", [3, 128, T], BF16)
    dbg = dscr("dbg", [T, 512], F32)

    es = contextlib.ExitStack()
    with es:
        S = Sched(nc, es)
        arena_t = es.enter_context(nc.sbuf_tensor("arena", [128, ARENA_COLS], F32))
        A = Arena(arena_t, ARENA_COLS)
        consts = es.enter_context(nc.sbuf_tensor("consts", [128, 1024], F32))
        ident_bf = consts[:, 0:64].bitcast(BF16)
        ident_f = consts[:, 64:192]
        iota_bf = consts[:, 192:256].bitcast(BF16)
        g1T = consts[:, 256:272]
        g2T = consts[:, 272:288]
        mk = consts[:, 288:290]
        cint = consts[:, 290:292].bitcast(U32)
        iota_f = consts[:, 320:448]
        pidx_f = consts[:, 448:576]
        iota16 = consts[:, 576:592]
        Bconst = Buf("consts")
        psum = [es.enter_context(nc.psum_tensor("ps%d" % i, [128, 512], F32)) for i in range(8)]
        Bps = [Buf("ps%d" % i) for i in range(8)]

        def psbf(i):
            return psum[i][:, :].bitcast(BF16)

        def psring(idx, bf=False):
            r = Ring([psbf(i) if bf else psum[i] for i in idx], "psr")
            r.bufs = [Bps[i] for i in idx]
            return r

        def dma_in(dst, src, Bdst, reads=(), carrier=None, eng="sp", **kw):
            S.op(eng, "dma_start", reads=list(reads), writes=[Bdst], dma=carrier or Bdst, out=dst, in_=src, **kw)

        S.op("pool", "iota", writes=[Bconst], out=iota_f, pattern=[[1, 128]], base=0, channel_multiplier=0,
             allow_small_or_imprecise_dtypes=True)
        S.op("pool", "iota", writes=[Bconst], out=pidx_f, pattern=[[0, 128]], base=0, channel_multiplier=1,
             allow_small_or_imprecise_dtypes=True)
        S.op("pool", "iota", writes=[Bconst], out=iota16, pattern=[[1, 16]], base=0, channel_multiplier=0,
             allow_small_or_imprecise_dtypes=True)
        S.op("pool", "iota", writes=[Bconst], out=cint[:, 0:1], pattern=[[0, 1]], base=4, channel_multiplier=0)
        S.op("pool", "iota", writes=[Bconst], out=cint[:, 1:2], pattern=[[0, 1]], base=15, channel_multiplier=0)
        S.op("dve", "tensor_tensor", reads=[Bconst], writes=[Bconst], out=ident_f, in0=iota_f, in1=pidx_f, op=ALU.is_equal)
        S.op("dve", "tensor_copy", reads=[Bconst], writes=[Bconst], out=ident_bf, in_=ident_f)
        S.op("dve", "tensor_copy", reads=[Bconst], writes=[Bconst], out=iota_bf, in_=iota_f)
        dma_in(g1T, mix_norm_g.rearrange("(k p) -> p k", p=128), Bconst, carrier=Buf("c1"), allow_slow_non_contiguous=True)
        dma_in(g2T, ffn_norm_g.rearrange("(k p) -> p k", p=128), Bconst, carrier=Buf("c2"), allow_slow_non_contiguous=True)
        dma_in(mk, masks[:, :], Bconst, carrier=Buf("c3"))

        Bwin = Buf("win_s")
        A.reset()
        wt_ring = Ring([A.f32(4096) for _ in range(2)], "wt")
        wb_ring = Ring([A.bf16(4096) for _ in range(2)], "wb")
        for k in range(16):
            wt, Bwt = wt_ring.next()
            wb, Bwb = wb_ring.next()
            dma_in(wt, w_in[k * 128:(k + 1) * 128, :], Bwt)
            S.op("dve", "tensor_scalar", reads=[Bwt, Bconst], writes=[Bwb], out=wb, in0=wt, scalar1=g1T[:, k:k + 1],
                 scalar2=None, op0=ALU.mult)
            S.op("pool", "dma_start", reads=[Bwb], writes=[Bwin], dma=Bwb, out=win_s[:, k, :], in_=wb)

        BUTs = Buf("UTs")
        BVs = Buf("Vs")
        S.barrier()
        if stop_after == "W":
            S.emit()
            return nc

        A.reset()
        wgv = A.bf16(16 * 1024)
        wgv3 = wgv.rearrange("p (k n) -> p k n", k=16)
        lng = A.f32(1024)
        lnb = A.f32(1024)
        bsr = A.f32(1024)
        wsT = A.bf16(1024)
        Bgw = Buf("gmlp_w")
        xt_ring = Ring([A.f32(D) for _ in range(2)], "xt")
        wsraw = xt_ring.aps[0][:, 0:1024]
        Bwsraw = xt_ring.bufs[0]
        dma_in(wgv3, win_s[:, :, 3072:4096], Bgw, reads=[Bwin], carrier=Buf("a1"))
        dma_in(lng, gmlp_ln_g.partition_broadcast(128), Bgw, carrier=Buf("a2"))
        dma_in(lnb, gmlp_ln_b.partition_broadcast(128), Bgw, carrier=Buf("a3"))
        dma_in(bsr, gmlp_b_s.rearrange("h p -> (h p)").partition_broadcast(128), Bgw, carrier=Buf("a4"))
        dma_in(wsraw.rearrange("p (h q) -> p h q", h=8), gmlp_w_s.rearrange("h p q -> p h q"), Bwsraw)
        for h in range(8):
            S.op("pe", "transpose", reads=[Bwsraw, Bconst], writes=[Bps[h // 4]],
                 out=psum[h // 4][:, (h % 4) * 128:(h % 4 + 1) * 128], in_=wsraw[:, h * 128:(h + 1) * 128], identity=ident_f)
        S.op("dve", "tensor_copy", reads=[Bps[0]], writes=[Bgw], out=wsT[:, 0:512], in_=psum[0][:, :])
        S.op("dve", "tensor_copy", reads=[Bps[1]], writes=[Bgw], out=wsT[:, 512:1024], in_=psum[1][:, :])

        xn_ring = Ring([A.bf16(D) for _ in range(4)], "xn")
        nT0 = A.bf16(16 * 512)
        nTs = [nT0, nT0]
        nT3s = [t.rearrange("p (k t) -> p k t", k=16) for t in nTs]
        BnT0 = Buf("nT0")
        BnTs = [BnT0, BnT0]
        wch_ring = Ring([A.bf16(2048) for _ in range(4)], "wch")
        stg_ring = Ring([A.f32(512) for _ in range(3)], "stg")
        guT = A.bf16(8 * 512)
        guT3 = guT.rearrange("p (h t) -> p h t", h=8)
        BguT = Buf("guT")
        gvg_ring = Ring([A.f32(1024) for _ in range(2)], "gvg")
        vtmp = A.f32(1024)
        Bvtmp = Buf("vtmp")
        v_ring = Ring([A.bf16(1024) for _ in range(2)], "v")
        mt_ring = Ring([A.f32(512) for _ in range(2)], "mt")
        bst_ring = Ring([A.bf16(8 * 512) for _ in range(1)], "bst")
        st_ring = Ring([A.f32(16) for _ in range(12)], "st")
        Bxr_own = [Buf("xro%d" % i) for i in range(8)]
        Bxr_oth = [Buf("xrx%d" % i) for i in range(8)]
        Bgg = [Buf("gg%d" % i) for i in range(8)]
        Bbout = Buf("boutT")
        ub_ring = Ring([A.bf16(D) for _ in range(2)], "ub")
        ut_ring = Ring([A.bf16(D) for _ in range(2)], "ut")
        vcar = Ring([None] * 16, "vcar")
        w3_state = [0]
        tp_ring = psring([0, 1], bf=True)
        ip_ring = psring([2, 3])
        gv_ring = psring([4, 5])
        mx_ring = psring([6, 7])

        def w3_chunk():
            c = w3_state[0]
            if c >= 128 or not need_uv:
                return
            w3_state[0] += 1
            ub, Bub = ub_ring.next()
            ut, But = ut_ring.next()
            dma_in(ub, peer_u[c * 128:(c + 1) * 128, :], Bub, eng="pool")
            for half in range(2):
                tp, Btp = tp_ring.next()
                for kk in range(8):
                    k = half * 8 + kk
                    S.op("pe", "transpose", reads=[Bub, Bconst], writes=[Btp], out=tp[:, kk * 128:(kk + 1) * 128],
                         in_=ub[:, k * 128:(k + 1) * 128], identity=ident_bf)
                if half == 0:
                    S.op("dve", "tensor_copy", reads=[Btp], writes=[But], out=ut[:, 0:1024], in_=tp)
                else:
                    S.op("act", "activation", reads=[Btp], writes=[But], out=ut[:, 1024:2048], in_=tp, func=AF.Copy)
            S.op("sp", "dma_start", reads=[But], writes=[], dma=But, out=UTs[c], in_=ut)

        def rms_to_bf16(xt, Bxt, xn, Bxn, st, Bst):
            S.op("act", "activation", reads=[Bxt], writes=[Bxn, Bst], out=xn, in_=xt, func=AF.Square, accum_out=st[:, 0:1])
            S.op("act", "activation", reads=[Bst], writes=[Bst], out=st[:, 1:2], in_=st[:, 0:1], func=AF.Sqrt,
                 scale=1.0 / D, bias=EPS)
            S.op("dve", "reciprocal", reads=[Bst], writes=[Bst], out=st[:, 2:3], in_=st[:, 1:2])
            S.op("act", "activation", reads=[Bxt, Bst], writes=[Bxn], out=xn, in_=xt, func=AF.Copy, scale=st[:, 2:3])

        def transpose16(xn, Bxn, ring, dst3, Bdst, col0):
            for half in range(2):
                tp, Btp = ring.next()
                for kk in range(8):
                    k = half * 8 + kk
                    S.op("pe", "transpose", reads=[Bxn, Bconst], writes=[Btp], out=tp[:, kk * 128:(kk + 1) * 128],
                         in_=xn[:, k * 128:(k + 1) * 128], identity=ident_bf)
                S.op("dve", "tensor_copy", reads=[Btp], writes=[Bdst], out=dst3[:, half * 8:(half + 1) * 8, col0:col0 + 128],
                     in_=tp.rearrange("p (k t) -> p k t", k=8))

        def prep(item):
            outs = []
            for src in item["tiles"]:
                xt, Bxt = xt_ring.next()
                xn, Bxn = xn_ring.next()
                st, Bst = st_ring.next()
                dma_in(xt, src, Bxt, eng="act")
                rms_to_bf16(xt, Bxt, xn, Bxn, st, Bst)
                outs.append((xn, Bxn))
            return outs

        def do_transposes(xns, slot):
            for i, (xn, Bxn) in enumerate(xns):
                transpose16(xn, Bxn, tp_ring, nT3s[slot], BnTs[slot], i * 128)
                w3_chunk()
                w3_chunk()

        def inproj_fm(cc, N, slot):
            wch, Bw = wch_ring.next()
            w3 = wch.rearrange("p (k c) -> p k c", k=16)
            dma_in(w3, win_s[:, :, cc * 128:(cc + 1) * 128], Bw, reads=[Bwin])
            ps, Bp = ip_ring.next()
            for k in range(16):
                S.op("pe", "matmul", reads=[Bw, BnTs[slot]], writes=[Bp], out=ps[:, 0:N], lhsT=w3[:, k, :],
                     rhs=nT3s[slot][:, k, 0:N], start=(k == 0), stop=(k == 15))
            return ps, Bp

        def main(item, slot):
            xrT, Bxr, own, blk = item["xrT"], item["Bxr"], item["own"], item["blk"]
            nT3, BnT = nT3s[slot], BnTs[slot]
            if blk is None:
                for cc in range(8):
                    ps, Bp = inproj_fm(cc, 128, slot)
                    stg, Bs = stg_ring.next()
                    S.op("act", "activation", reads=[Bp], writes=[Bs], out=stg[:, 0:128], in_=ps[:, 0:128], func=AF.Copy)
                    S.op("pool", "dma_start", reads=[Bs], writes=[Bxr[cc]], dma=Bs, out=xrT[cc * 128:(cc + 1) * 128, 0:2],
                         in_=stg[:, 0:2], allow_slow_non_contiguous=True)
                    S.op("pool", "dma_start", reads=[Bs], writes=[Bxr[cc]], dma=Bs, out=xrT[cc * 128:(cc + 1) * 128, 4098:4099],
                         in_=stg[:, 2:3], allow_slow_non_contiguous=True)
                return
            c0 = 2 + blk * 512
            for cc in range(8):
                ps, Bp = inproj_fm(cc, 512, slot)
                stg, Bs = stg_ring.next()
                S.op("act", "activation", reads=[Bp], writes=[Bs], out=stg, in_=ps[:, :], func=AF.Copy)
                S.op("pool", "dma_start", reads=[Bs], writes=[Bxr[cc]], dma=Bs,
                     out=xrT[cc * 128:(cc + 1) * 128, c0:c0 + 512], in_=stg)
            if not own:
                return
            for cc in range(8, 16):
                ps, Bp = inproj_fm(cc, 512, slot)
                stg, Bs = stg_ring.next()
                S.op("act", "activation", reads=[Bp], writes=[Bs], out=stg, in_=ps[:, :], func=GELU)
                S.op("pool", "dma_start", reads=[Bs], writes=[Bgg[cc - 8]], dma=Bs,
                     out=ggT[(cc - 8) * 128:(cc - 7) * 128, blk * 512:(blk + 1) * 512], in_=stg)
            for cc in range(16, 24):
                ps, Bp = inproj_fm(cc, 512, slot)
                S.op("act", "activation", reads=[Bp], writes=[BguT], out=guT3[:, cc - 16, :], in_=ps[:, :], func=GELU)
            bst, Bbst = bst_ring.next()
            bst3 = bst.rearrange("p (h t) -> p h t", h=8)
            vs = {}

            def gv_part(i):
                gvg, Bgvg = gvg_ring.next()
                for nb in range(2):
                    ps, Bp = gv_ring.next()
                    for k in range(16):
                        S.op("pe", "matmul", reads=[BnT, Bgw], writes=[Bp], out=ps[:, :],
                             lhsT=nT3[:, k, i * 128:(i + 1) * 128], rhs=wgv3[:, k, nb * 512:(nb + 1) * 512],
                             start=(k == 0), stop=(k == 15))
                    S.op("act", "activation", reads=[Bp], writes=[Bgvg], out=gvg[:, nb * 512:(nb + 1) * 512],
                         in_=ps[:, :], func=GELU)
                st, Bst = st_ring.next()
                S.op("dve", "bn_stats", reads=[Bgvg], writes=[Bst], out=st[:, 0:6], in_=gvg[:, 0:512])
                S.op("dve", "bn_stats", reads=[Bgvg], writes=[Bst], out=st[:, 6:12], in_=gvg[:, 512:1024])
                S.op("dve", "bn_aggr", reads=[Bst], writes=[Bst], out=st[:, 12:14], in_=st[:, 0:12])
                S.op("act", "activation", reads=[Bst], writes=[Bst], out=st[:, 14:15], in_=st[:, 13:14], func=AF.Sqrt,
                     bias=EPS)
                S.op("dve", "reciprocal", reads=[Bst], writes=[Bst], out=st[:, 15:16], in_=st[:, 14:15])
                S.op("dve", "tensor_scalar", reads=[Bgvg, Bst], writes=[Bvtmp], out=vtmp, in0=gvg, scalar1=st[:, 12:13],
                     scalar2=st[:, 15:16], op0=ALU.subtract, op1=ALU.mult)
                S.op("pool", "tensor_tensor", reads=[Bvtmp, Bgw], writes=[Bvtmp], out=vtmp, in0=vtmp, in1=lng, op=ALU.mult)
                v, Bv = v_ring.next()
                S.op("pool", "tensor_tensor", reads=[Bvtmp, Bgw], writes=[Bv], out=v, in0=vtmp, in1=lnb, op=ALU.add)
                vs[i] = (v, Bv)

            def mix_part(i):
                v, Bv = vs[i]
                for hh in range(2):
                    ps, Bp = mx_ring.next()
                    for h4 in range(4):
                        h = hh * 4 + h4
                        S.op("pe", "matmul", reads=[Bv, Bgw], writes=[Bp], out=ps[:, h4 * 128:(h4 + 1) * 128],
                             lhsT=v[:, h * 128:(h + 1) * 128], rhs=wsT[:, h * 128:(h + 1) * 128], start=True, stop=True)
                    mt, Bmt = mt_ring.next()
                    S.op("dve", "tensor_tensor", reads=[Bp, Bgw], writes=[Bmt], out=mt, in0=ps[:, :],
                         in1=bsr[:, hh * 512:(hh + 1) * 512], op=ALU.add)
                    S.op("dve", "tensor_tensor", reads=[Bmt, BguT], writes=[Bbst],
                         out=bst3[:, hh * 4:(hh + 1) * 4, i * 128:(i + 1) * 128],
                         in0=mt.rearrange("p (h t) -> p h t", h=4),
                         in1=guT3[:, hh * 4:(hh + 1) * 4, i * 128:(i + 1) * 128], op=ALU.mult)

            gv_part(0)
            for i in range(1, 4):
                gv_part(i)
                mix_part(i - 1)
            mix_part(3)
            S.op("pool", "dma_start", reads=[Bbst], writes=[Bbout], dma=Bbst,
                 out=boutT.rearrange("(h c) t -> c h t", h=8)[:, :, blk * 512:(blk + 1) * 512], in_=bst3)

        items = []
        for (x_src, x_hal, xrT, Bxr, own) in ((x_oth, x_ohalo, xrT_oth, Bxr_oth, False), (x_own, x_halo, xrT_own, Bxr_own, True)):
            items.append(dict(tiles=[x_hal[:, :]], xrT=xrT, Bxr=Bxr, own=own, blk=None))
            for blk in range(8):
                items.append(dict(tiles=[x_src[blk * 512 + i * 128:blk * 512 + (i + 1) * 128, :] for i in range(4)],
                                  xrT=xrT, Bxr=Bxr, own=own, blk=blk))
        pre = prep(items[0])
        do_transposes(pre, 0)
        for n, item in enumerate(items):
            nxt = prep(items[n + 1]) if n + 1 < len(items) else None
            main(item, n % 2)
            if nxt is not None:
                do_transposes(nxt, (n + 1) % 2)
        while w3_state[0] < 128 and need_uv:
            w3_chunk()
        S.barrier()
        if stop_after == "A":
            S.emit()
            return nc

        A.reset()
        wg = A.bf16(6 * 8 * 128)
        wg4 = wg.rearrange("p (g h c) -> p g h c", g=6, h=8)
        Bwg = Buf("wg")
        for gi, src in enumerate([lru_w_a[0], lru_w_x[0], lru_w_a[1], lru_w_x[1], oth_w_a, oth_w_x]):
            dma_in(wg4[:, gi], src.rearrange("h ci co -> ci h co"), Bwg, carrier=Buf("bw%d" % gi), eng="pool")
        pp = A.f32(256)
        Bpp = Buf("pp")
        nslow = dict(allow_slow_non_contiguous=True)
        dma_in(pp[:, 0:16].rearrange("p (d h) -> p d h", d=2), lru_b_a.rearrange("d (h p) -> p d h", p=128), Bpp,
               carrier=Buf("bp0"), **nslow)
        dma_in(pp[:, 16:24], oth_b_a.rearrange("(h p) -> p h", p=128), Bpp, carrier=Buf("bp1"), **nslow)
        dma_in(pp[:, 24:40].rearrange("p (d h) -> p d h", d=2), lru_b_x.rearrange("d (h p) -> p d h", p=128), Bpp,
               carrier=Buf("bp2"), **nslow)
        dma_in(pp[:, 40:48], oth_b_x.rearrange("(h p) -> p h", p=128), Bpp, carrier=Buf("bp3"), **nslow)
        dma_in(pp[:, 48:64].rearrange("p (d h) -> p d h", d=2), lru_lambda.rearrange("d (h p) -> p d h", p=128), Bpp,
               carrier=Buf("bp4"), **nslow)
        dma_in(pp[:, 64:72], oth_lam.rearrange("(h p) -> p h", p=128), Bpp, carrier=Buf("bp5"), **nslow)
        dma_in(pp[:, 144:176].rearrange("p (k h) -> p k h", k=4), conv_w.rearrange("k (h p) -> p k h", p=128), Bpp,
               carrier=Buf("bp6"), **nslow)
        dma_in(pp[:, 176:184], conv_b.rearrange("(h p) -> p h", p=128), Bpp, carrier=Buf("bp7"), **nslow)
        S.op("act", "activation", reads=[Bpp], writes=[Bpp], out=pp[:, 120:144], in_=pp[:, 48:72], func=AF.Exp, scale=-1.0)
        S.op("act", "activation", reads=[Bpp], writes=[Bpp], out=pp[:, 120:144], in_=pp[:, 120:144], func=AF.Ln, bias=1.0)
        S.op("dve", "tensor_scalar", reads=[Bpp], writes=[Bpp], out=pp[:, 72:96], in0=pp[:, 120:144], scalar1=-8.0, scalar2=None,
             op0=ALU.mult)
        S.op("dve", "tensor_scalar", reads=[Bpp], writes=[Bpp], out=pp[:, 96:120], in0=pp[:, 120:144], scalar1=-16.0,
             scalar2=None, op0=ALU.mult)
        diag = A.f32(4 * 128)
        Bdiag = Buf("diag")
        XR = A.f32(4100)
        XC = A.f32(T)
        XCb = A.bf16(T)
        rim_aps = [[A.f32(2048) for _ in range(3)] for _ in range(2)]
        rim_bufs = [[Buf("rim%d_%d" % (r, j)) for j in range(3)] for r in range(2)]
        rim_i = [0]
        A1 = A.f32(T)
        U1 = A.f32(T)
        A2 = A.f32(T)
        U2 = A.f32(T)
        OB = XCb
        BXR, BA1, BU1, BA2, BU2, BOB = [Buf(n) for n in "XR A1 U1 A2 U2 OB".split()]
        BXC = [Buf("XC%d" % i) for i in range(8)]
        BXCb = [Buf("XCb%d" % i) for i in range(8)]
        Baout = Buf("aoutT")
        gps = psring([0, 1, 2, 3, 4, 5, 6, 7])

        def conv(hd, xrT, Bxr):
            dma_in(XR[:, 0:4099], xrT[hd * 128:(hd + 1) * 128, 0:4099], BXR, reads=[Bxr[hd]])
            S.op("dve", "tensor_scalar", reads=[BXR, Bpp], writes=BXC, out=XC, in0=XR[:, 0:T],
                 scalar1=pp[:, 144 + hd:145 + hd], scalar2=pp[:, 176 + hd:177 + hd], op0=ALU.mult, op1=ALU.add)
            for k in range(1, 4):
                S.op("dve", "scalar_tensor_tensor", reads=[BXR, Bpp] + BXC, writes=BXC, out=XC, in0=XR[:, k:k + T],
                     scalar=pp[:, 144 + k * 8 + hd:145 + k * 8 + hd], in1=XC, op0=ALU.mult, op1=ALU.add)
            for h2 in range(2):
                S.op("act", "activation", reads=BXC, writes=BXCb[h2 * 4:h2 * 4 + 4], out=XCb[:, h2 * 2048:(h2 + 1) * 2048],
                     in_=XC[:, h2 * 2048:(h2 + 1) * 2048], func=AF.Copy)

        def gates(hd, g, Aa, BAa, Uu, BUu):
            col = g * 8 + hd
            for h2 in range(2):
                r = rim_i[0] % 2
                rim_i[0] += 1
                Rt, It, Mt = rim_aps[r]
                BRt, BIt, BMt = rim_bufs[r]
                for nb in range(4):
                    t0 = h2 * 2048 + nb * 512
                    ps, Bp = gps.next()
                    S.op("pe", "matmul", reads=[Bwg, BXCb[h2 * 4 + nb]], writes=[Bp], out=ps[:, :], lhsT=wg4[:, 2 * g, hd, :],
                         rhs=XCb[:, t0:t0 + 512], start=True, stop=True)
                    S.op("act", "activation", reads=[Bp, Bpp], writes=[BRt], out=Rt[:, nb * 512:(nb + 1) * 512], in_=ps[:, :],
                         func=AF.Sigmoid, bias=pp[:, col:col + 1])
                    ps, Bp = gps.next()
                    S.op("pe", "matmul", reads=[Bwg, BXCb[h2 * 4 + nb]], writes=[Bp], out=ps[:, :], lhsT=wg4[:, 2 * g + 1, hd, :],
                         rhs=XCb[:, t0:t0 + 512], start=True, stop=True)
                    S.op("act", "activation", reads=[Bp, Bpp], writes=[BIt], out=It[:, nb * 512:(nb + 1) * 512], in_=ps[:, :],
                         func=AF.Sigmoid, bias=pp[:, 24 + col:25 + col])
                sl = slice(h2 * 2048, (h2 + 1) * 2048)
                S.op("act", "activation", reads=[BRt, Bpp], writes=[BAa], out=Aa[:, sl], in_=Rt, func=AF.Exp,
                     scale=pp[:, 72 + col:73 + col])
                S.op("pool", "tensor_tensor", reads=[BAa], writes=[BMt], out=Mt, in0=Aa[:, sl], in1=Aa[:, sl], op=ALU.mult)
                S.op("act", "activation", reads=[BMt], writes=[BMt], out=Mt, in_=Mt, func=AF.Sqrt, scale=-1.0, bias=1.0)
                S.op("pool", "tensor_tensor", reads=[BMt, BIt], writes=[BMt], out=Mt, in0=Mt, in1=It, op=ALU.mult)
                S.op("dve", "tensor_tensor", reads=[BMt] + BXC[h2 * 4:h2 * 4 + 4], writes=[BUu], out=Uu[:, sl], in0=Mt,
                     in1=XC[:, sl], op=ALU.mult)

        for hd in range(8):
            if need_uv:
                for c in range(hd * 16, hd * 16 + 16):
                    _, Bvc = vcar.next()
                    S.op("pool", "dma_start", reads=[], writes=[], dma=Bvc, out=Vs[c * 128:(c + 1) * 128, :],
                         in_=peer_v[c * 128:(c + 1) * 128, :])
            conv(hd, xrT_oth, Bxr_oth)
            gates(hd, 2, A1, BA1, U1, BU1)
            S.op("dve", "tensor_tensor_scan", reads=[BA1, BU1], writes=[BA2], out=A2, data0=A1, data1=U1, initial=0.0,
                 op0=ALU.mult, op1=ALU.add)
            S.op("dve", "tensor_tensor_scan", reads=[BA1, BU1], writes=[BU2], out=U2[:, T - 1::-1], data0=A1[:, T - 1::-1],
                 data1=U1[:, T - 1::-1], initial=0.0, op0=ALU.mult, op1=ALU.add)
            S.op("dve", "tensor_tensor", reads=[BA2, Bconst], writes=[Bpp], out=pp[:, 184 + hd:185 + hd], in0=A2[:, T - 1:T],
                 in1=mk[:, 0:1], op=ALU.mult)
            S.op("dve", "tensor_tensor", reads=[BU2, Bconst], writes=[Bpp], out=pp[:, 192 + hd:193 + hd], in0=U2[:, 0:1],
                 in1=mk[:, 1:2], op=ALU.mult)
            conv(hd, xrT_own, Bxr_own)
            gates(hd, 0, A1, BA1, U1, BU1)
            gates(hd, 1, A2, BA2, U2, BU2)
            S.op("dve", "tensor_tensor_scan", reads=[BA1, BU1, Bpp], writes=[BU1], out=U1, data0=A1, data1=U1,
                 initial=pp[:, 184 + hd:185 + hd], op0=ALU.mult, op1=ALU.add)
            S.op("dve", "tensor_tensor_scan", reads=[BA2, BU2, Bpp], writes=[BU2], out=U2[:, T - 1::-1], data0=A2[:, T - 1::-1],
                 data1=U2[:, T - 1::-1], initial=pp[:, 192 + hd:193 + hd], op0=ALU.mult, op1=ALU.add)
            S.op("pool", "tensor_tensor", reads=[BU1, BU2], writes=[BU1], out=U1, in0=U1, in1=U2, op=ALU.add)
            dma_in(XR[:, 0:T], ggT[hd * 128:(hd + 1) * 128, :], BXR, reads=[Bgg[hd]])
            S.op("dve", "tensor_tensor", reads=[BU1, BXR], writes=[BOB] + BXCb, out=OB, in0=U1, in1=XR[:, 0:T], op=ALU.mult)
            S.op("pool", "dma_start", reads=[BOB] + BXCb, writes=[Baout], dma=BOB, out=aoutT[hd * 128:(hd + 1) * 128, :], in_=OB)
        S.barrier()
        if stop_after == "B":
            S.emit()
            return nc

        A.reset()
        wo = A.bf16(16 * D)
        wo3 = wo.rearrange("p (k n) -> p k n", k=16)
        Bwo = Buf("wo")
        dma_in(wo3, w_out.rearrange("(k p) n -> p k n", p=128), Bwo, eng="pool")
        ab_ring = Ring([A.bf16(16 * 128) for _ in range(2)], "ab")
        xt_ring = Ring([A.f32(D) for _ in range(2)], "xt")
        h1_ring = Ring([A.f32(D) for _ in range(2)], "h1")
        xn_ring = Ring([A.bf16(D) for _ in range(3)], "xn")
        n2t_ring = Ring([A.bf16(16 * 128) for _ in range(2)], "n2t")
        st_ring = Ring([A.f32(16) for _ in range(4)], "st")
        g2r = A.f32(D)
        Bg2r = Buf("g2r")
        dma_in(g2r, ffn_norm_g.partition_broadcast(128), Bg2r)
        Bh1s = [Buf("h1s%d" % i) for i in range(NT)]
        Bn2Ts = [Buf("n2Ts%d" % i) for i in range(NT // 2)]
        tp_ring = psring([4, 5, 6, 7], bf=True)
        aoutT3 = aoutT.rearrange("(h c) t -> c h t", h=8)
        boutT3 = boutT.rearrange("(h c) t -> c h t", h=8)
        abcar = [[Buf("abA0"), Buf("abA1")], [Buf("abB0"), Buf("abB1")]]
        pend_c1 = [None]
        for i in range(NT):
            ab, Bab = ab_ring.next()
            ab3 = ab.rearrange("p (k t) -> p k t", k=16)
            dma_in(ab3[:, 0:8, :], aoutT3[:, :, i * 128:(i + 1) * 128], Bab, reads=[Baout], carrier=abcar[0][i % 2])
            dma_in(ab3[:, 8:16, :], boutT3[:, :, i * 128:(i + 1) * 128], Bab, reads=[Bbout], carrier=abcar[1][i % 2])
            xt, Bxt = xt_ring.next()
            dma_in(xt, x_own[i * 128:(i + 1) * 128, :], Bxt)
            h1, Bh1 = h1_ring.next()
            for nb in range(4):
                for k in range(16):
                    S.op("pe", "matmul", reads=[Bab, Bwo], writes=[Bps[nb]], out=psum[nb][:, :], lhsT=ab3[:, k, :],
                         rhs=wo3[:, k, nb * 512:(nb + 1) * 512], start=(k == 0), stop=(k == 15))
                S.op("dve", "tensor_tensor", reads=[Bps[nb], Bxt], writes=[Bh1], out=h1[:, nb * 512:(nb + 1) * 512],
                     in0=psum[nb][:, :], in1=xt[:, nb * 512:(nb + 1) * 512], op=ALU.add)
            S.op("pool", "dma_start", reads=[Bh1], writes=[Bh1s[i]], dma=Bh1, out=h1s[i * 128:(i + 1) * 128, :], in_=h1)
            xn, Bxn = xn_ring.next()
            st, Bst = st_ring.next()
            rms_to_bf16(h1, Bh1, xn, Bxn, st, Bst)
            S.op("pool", "tensor_tensor", reads=[Bxn, Bg2r], writes=[Bxn], out=xn, in0=xn, in1=g2r, op=ALU.mult)
            if pend_c1[0] is not None:
                pend_c1[0]()

            def fin(i=i, xn=xn, Bxn=Bxn):
                n2t, Bn2t = n2t_ring.next()
                n2t3 = n2t.rearrange("p (k t) -> p k t", k=16)
                transpose16(xn, Bxn, tp_ring, n2t3, Bn2t, 0)
                S.op("pool", "dma_start", reads=[Bn2t], writes=[Bn2Ts[i // 2]], dma=Bn2t, out=n2Ts[:, :, i * 128:(i + 1) * 128],
                     in_=n2t3)
            pend_c1[0] = fin
        pend_c1[0]()
        S.barrier()

        A.reset()
        wq = A.bf16(16 * D)
        wq3 = wq.rearrange("p (k n) -> p k n", k=16)
        Bwq = Buf("wq")
        dma_in(wq3, peer_w_q.rearrange("(k p) n -> p k n", p=128), Bwq, eng="pool")
        kraw = A.f32(16 * 128)
        keysT = A.bf16(16 * 128)
        keysT3 = keysT.rearrange("p (c n) -> p c n", c=16)
        Bkeys = Buf("keys")
        dma_in(kraw.rearrange("p (c k) -> p c k", c=16), peer_sub_keys.rearrange("h p n k -> n (h p) k"), Bkeys,
               carrier=Buf("kr"))
        for cc in range(16):
            S.op("pe", "transpose", reads=[Bkeys, Bconst], writes=[Bps[cc // 4]],
                 out=psum[cc // 4][:, (cc % 4) * 128:(cc % 4 + 1) * 128], in_=kraw[:, cc * 128:(cc + 1) * 128], identity=ident_f)
        for b4 in range(4):
            S.op("dve", "tensor_copy", reads=[Bps[b4]], writes=[Bkeys], out=keysT[:, b4 * 512:(b4 + 1) * 512], in_=psum[b4][:, :])
        n2b_ring = Ring([A.bf16(16 * 512) for _ in range(1)], "n2b")
        qT = A.bf16(16 * 512)
        qT3 = qT.rearrange("p (c t) -> p c t", c=16)
        BqT = Buf("qT")
        sc_ring = Ring([A.f32(D) for _ in range(2)], "sc")
        V01 = A.f32(256)
        I01u = A.u32(256)
        I01f = A.f32(256)
        wk16 = kraw
        cand = A.f32(8 * 256)
        wk2 = A.f32(8 * 256)
        Ex = A.f32(128)
        Cv = A.f32(128)
        Pu = A.u32(128)
        Au = A.u32(128)
        Bu_ = A.u32(128)
        Af = A.f32(128)
        Bf = A.f32(128)
        eqA = A.f32(8 * 256)
        eqB = A.f32(8 * 256)
        sel_ring = Ring([A.f32(3 * 128) for _ in range(2)], "sel")
        selb_ring = Ring([A.bf16(3 * 128) for _ in range(2)], "selb")
        nC0 = A.f32(8)
        Zs = A.f32(8)
        Btk = Buf("topk_tmp")
        BselT = [Buf("selT%d" % i) for i in range(NT // 2)]
        dbgcar = [Buf("dbg0"), Buf("dbg1")]
        qps = psring([4, 5])
        sps = [0, 1, 2, 3]
        tps = psring([6, 7])
        V4 = V01.rearrange("p (s h k) -> p s h k", s=2, h=8)
        I4u = I01u.rearrange("p (s h k) -> p s h k", s=2, h=8)
        I4f = I01f.rearrange("p (s h k) -> p s h k", s=2, h=8)
        cand4 = cand.rearrange("p (h a b) -> p h a b", h=8, a=16)
        eqA4 = eqA.rearrange("p (h k a) -> p h k a", h=8, k=16)
        eqB4 = eqB.rearrange("p (h k a) -> p h k a", h=8, k=16)
        mkb = lambda n: [[Buf("%s%d%d" % (n, h, p)) for p in range(2)] for h in range(8)]
        Bv8, Bi8, Bwk16, Bv8b, Bi8b = mkb("v8"), mkb("i8"), mkb("wk16"), mkb("v8b"), mkb("i8b")
        mk1 = lambda n: [Buf("%s%d" % (n, h)) for h in range(8)]
        Bc8, Bp8, Bwk2, Bc8b, Bp8b = mk1("c8"), mk1("p8"), mk1("wk2"), mk1("c8b"), mk1("p8b")
        Bcand, BI01f, BAu, BBu, BAf, BBf, BEx, BZs, BeqA, BeqB = [Buf(n) for n in "cand I01f Au Bu Af Bf Ex Zs eqA eqB".split()]
        C3 = Cv.rearrange("p (h k) -> p h k", h=8)
        Pu3 = Pu.rearrange("p (h k) -> p h k", h=8)
        for blk in range(8):
            n2b, Bn2b = n2b_ring.next()
            n2b3 = n2b.rearrange("p (k t) -> p k t", k=16)
            dma_in(n2b3, n2Ts[:, :, blk * 512:(blk + 1) * 512], Bn2b, reads=[Bn2Ts[2 * blk], Bn2Ts[2 * blk + 1]])
            for cc in range(16):
                ps, Bp = qps.next()
                for k in range(16):
                    S.op("pe", "matmul", reads=[Bwq, Bn2b], writes=[Bp], out=ps[:, :], lhsT=wq3[:, k, cc * 128:(cc + 1) * 128],
                         rhs=n2b3[:, k, :], start=(k == 0), stop=(k == 15))
                S.op("act", "activation", reads=[Bp], writes=[BqT], out=qT3[:, cc, :], in_=ps[:, :], func=AF.Copy)
            for i in range(4):
                tile = blk * 4 + i
                sc, Bsc = sc_ring.next()
                for cc in range(16):
                    b4 = sps[cc // 4]
                    S.op("pe", "matmul", reads=[BqT, Bkeys], writes=[Bps[b4]], out=psum[b4][:, (cc % 4) * 128:(cc % 4 + 1) * 128],
                         lhsT=qT3[:, cc, i * 128:(i + 1) * 128], rhs=keysT3[:, cc, :], start=True, stop=True)
                for b4 in range(4):
                    S.op("act", "activation", reads=[Bps[sps[b4]]], writes=[Bsc], out=sc[:, b4 * 512:(b4 + 1) * 512],
                         in_=psum[sps[b4]][:, :], func=AF.Copy)
                hp = [(h, p) for h in range(8) for p in range(2)]
                srcs = {(h, p): sc[:, (h * 2 + p) * 128:(h * 2 + p + 1) * 128] for (h, p) in hp}
                for (h, p) in hp:
                    S.op("dve", "max", reads=[Bsc], writes=[Bv8[h][p]], out=V4[:, p, h, 0:8], in_=srcs[(h, p)])
                for (h, p) in hp:
                    S.op("dve", "max_index", reads=[Bsc, Bv8[h][p]], writes=[Bi8[h][p]], out=I4u[:, p, h, 0:8],
                         in_max=V4[:, p, h, 0:8], in_values=srcs[(h, p)])
                for (h, p) in hp:
                    S.op("dve", "match_replace", reads=[Bsc, Bv8[h][p]], writes=[Bwk16[h][p]], out=wk16[:, (h * 2 + p) * 128:(h * 2 + p + 1) * 128],
                         in_to_replace=V4[:, p, h, 0:8], in_values=srcs[(h, p)], imm_value=-1e30)
                for (h, p) in hp:
                    S.op("dve", "max", reads=[Bwk16[h][p]], writes=[Bv8b[h][p]], out=V4[:, p, h, 8:16],
                         in_=wk16[:, (h * 2 + p) * 128:(h * 2 + p + 1) * 128])
                for (h, p) in hp:
                    S.op("dve", "max_index", reads=[Bwk16[h][p], Bv8b[h][p]], writes=[Bi8b[h][p]], out=I4u[:, p, h, 8:16],
                         in_max=V4[:, p, h, 8:16], in_values=wk16[:, (h * 2 + p) * 128:(h * 2 + p + 1) * 128])
                allv = [Bv8[h][p] for (h, p) in hp] + [Bv8b[h][p] for (h, p) in hp]
                alli = [Bi8[h][p] for (h, p) in hp] + [Bi8b[h][p] for (h, p) in hp]
                S.op("dve", "tensor_tensor", reads=allv, writes=[Bcand], out=cand4,
                     in0=V4[:, 0].unsqueeze(3).to_broadcast([128, 8, 16, 16]),
                     in1=V4[:, 1].unsqueeze(2).to_broadcast([128, 8, 16, 16]), op=ALU.add)
                S.op("dve", "tensor_copy", reads=alli, writes=[BI01f], out=I01f, in_=I01u)
                for h in range(8):
                    S.op("dve", "max", reads=[Bcand], writes=[Bc8[h]], out=C3[:, h, 0:8], in_=cand[:, h * 256:(h + 1) * 256])
                for h in range(8):
                    S.op("dve", "max_index", reads=[Bcand, Bc8[h]], writes=[Bp8[h]], out=Pu3[:, h, 0:8], in_max=C3[:, h, 0:8],
                         in_values=cand[:, h * 256:(h + 1) * 256])
                for h in range(8):
                    S.op("dve", "match_replace", reads=[Bcand, Bc8[h]], writes=[Bwk2[h]], out=wk2[:, h * 256:(h + 1) * 256],
                         in_to_replace=C3[:, h, 0:8], in_values=cand[:, h * 256:(h + 1) * 256], imm_value=-1e30)
                for h in range(8):
                    S.op("dve", "max", reads=[Bwk2[h]], writes=[Bc8b[h]], out=C3[:, h, 8:16], in_=wk2[:, h * 256:(h + 1) * 256])
                for h in range(8):
                    S.op("dve", "max_index", reads=[Bwk2[h], Bc8b[h]], writes=[Bp8b[h]], out=Pu3[:, h, 8:16], in_max=C3[:, h, 8:16],
                         in_values=wk2[:, h * 256:(h + 1) * 256])
                allc = Bc8 + Bc8b
                allp = Bp8 + Bp8b
                S.op("dve", "tensor_tensor", reads=allp + [Bconst], writes=[BAu], out=Au, in0=Pu,
                     in1=cint[:, 0:1].to_broadcast([128, 128]), op=ALU.logical_shift_right)
                S.op("dve", "tensor_tensor", reads=allp + [Bconst], writes=[BBu], out=Bu_, in0=Pu,
                     in1=cint[:, 1:2].to_broadcast([128, 128]), op=ALU.bitwise_and)
                sel, Bsel = sel_ring.next()
                sel4 = sel.rearrange("p (s h k) -> p s h k", s=3, h=8)
                S.op("dve", "tensor_tensor", reads=allc, writes=[BEx], out=Ex.rearrange("p (h k) -> p h k", h=8), in0=C3,
                     in1=C3[:, :, 0:1].to_broadcast([128, 8, 16]), op=ALU.subtract)
                S.op("dve", "tensor_copy", reads=[BAu], writes=[BAf], out=Af, in_=Au)
                S.op("dve", "tensor_copy", reads=[BBu], writes=[BBf], out=Bf, in_=Bu_)
                S.op("act", "activation", reads=[BEx], writes=[BEx], out=Ex, in_=Ex, func=AF.Exp)
                for XF, BXF, e4, Be, s_ in ((Af, BAf, eqA4, BeqA, 0), (Bf, BBf, eqB4, BeqB, 1)):
                    S.op("dve", "tensor_tensor", reads=[BXF, Bconst], writes=[Be], out=e4,
                         in0=XF.rearrange("p (h k) -> p h k", h=8).unsqueeze(3).to_broadcast([128, 8, 16, 16]),
                         in1=iota16.unsqueeze(1).unsqueeze(1).to_broadcast([128, 8, 16, 16]), op=ALU.is_equal)
                for XF, BXF, e4, Be, s_ in ((Af, BAf, eqA4, BeqA, 0), (Bf, BBf, eqB4, BeqB, 1)):
                    S.op("dve", "tensor_tensor", reads=[Be, BI01f], writes=[Be], out=e4, in0=e4,
                         in1=I4f[:, s_].unsqueeze(2).to_broadcast([128, 8, 16, 16]), op=ALU.mult)
                S.op("dve", "tensor_reduce", reads=[BEx], writes=[BZs], out=Zs, in_=Ex.rearrange("p (h k) -> p h k", h=8),
                     axis=AX.X, op=ALU.add)
                S.op("dve", "reciprocal", reads=[BZs], writes=[BZs], out=Zs, in_=Zs)
                S.op("dve", "tensor_reduce", reads=[BeqA], writes=[Bsel], out=sel4[:, 0], in_=eqA4, axis=AX.X, op=ALU.add)
                S.op("dve", "tensor_reduce", reads=[BeqB], writes=[Bsel], out=sel4[:, 1], in_=eqB4, axis=AX.X, op=ALU.add)
                S.op("dve", "tensor_tensor", reads=[BEx, BZs], writes=[Bsel], out=sel4[:, 2], in0=Ex.rearrange("p (h k) -> p h k", h=8),
                     in1=Zs.unsqueeze(2).to_broadcast([128, 8, 16]), op=ALU.mult)
                if "dbg" in debug:
                    S.op("pool", "dma_start", reads=[Bsel], writes=[], dma=dbgcar[tile % 2],
                         out=dbg[tile * 128:(tile + 1) * 128, 0:384], in_=sel)
                selb, Bselb = selb_ring.next()
                ps, Bp = tps.next()
                for s_ in range(3):
                    S.op("pe", "transpose", reads=[Bsel, Bconst], writes=[Bp], out=ps[:, s_ * 128:(s_ + 1) * 128],
                         in_=sel[:, s_ * 128:(s_ + 1) * 128], identity=ident_f)
                S.op("act", "activation", reads=[Bp], writes=[Bselb], out=selb, in_=ps[:, 0:384], func=AF.Copy)
                S.op("pool", "dma_start", reads=[Bselb], writes=[BselT[tile // 2]], dma=Bselb,
                     out=selT.rearrange("s p t -> p s t")[:, :, tile * 128:(tile + 1) * 128],
                     in_=selb.rearrange("p (s t) -> p s t", s=3))
        S.barrier()
        if stop_after == "C":
            S.emit()
            return nc

        A.reset()
        TB = 256
        G = A.bf16(128 * TB)
        G3 = G.rearrange("p (c t) -> p c t", c=128)
        BG = Buf("G")
        fg = A.f32(D)
        Bfg = Buf("fg")
        dma_in(fg, final_norm_g.partition_broadcast(128), Bfg)
        tab_ring = Ring([A.bf16(3 * TB) for _ in range(2)], "tab")
        n2d_ring = Ring([A.bf16(16 * TB) for _ in range(2)], "n2d")
        pq_aps = [A.bf16(2 * 16 * 128) for _ in range(2)]
        pq_bufs = [[(Buf("p%d_%d" % (r, t)), Buf("q%d_%d" % (r, t))) for t in range(16)] for r in range(2)]
        ut_ring = Ring([A.bf16(D) for _ in range(6)], "utd")
        vt_ring = Ring([A.bf16(1024) for _ in range(8)], "vtd")
        gt_ring = Ring([A.bf16(TB) for _ in range(2)], "gt")
        hf_ring = Ring([A.f32(D) for _ in range(2)], "hf")
        sqj = A.bf16(D)
        Bsqj = Buf("sqj")
        st_ring = Ring([A.f32(16) for _ in range(4)], "st")
        gps2 = psring([0, 1])
        xps = psring([2, 3])
        Bout = Buf("out")
        for tb in range(T // TB):
            t0 = tb * TB
            tab, Btab = tab_ring.next()
            tab3 = tab.rearrange("p (s t) -> p s t", s=3)
            dma_in(tab3, selT.rearrange("s p t -> p s t")[:, :, t0:t0 + TB], Btab, reads=[BselT[tb]])
            n2d, Bn2d = n2d_ring.next()
            n2d3 = n2d.rearrange("p (k t) -> p k t", k=16)
            dma_in(n2d3, n2Ts[:, :, t0:t0 + TB], Bn2d, reads=[Bn2Ts[tb]])
            for sb in range(TB // 16):
                pq = pq_aps[sb % 2]
                pq4 = pq.rearrange("p (s t i) -> p s t i", s=2, t=16)
                for tt in range(16):
                    t = sb * 16 + tt
                    BP_, BQ_ = pq_bufs[sb % 2][tt]
                    S.op("dve", "tensor_scalar", reads=[Btab, Bconst], writes=[BP_], out=pq4[:, 0, tt, :], in0=iota_bf,
                         scalar1=tab3[:, 0, t:t + 1], scalar2=None, op0=ALU.is_equal)
                    S.op("dve", "tensor_scalar", reads=[Btab, Bconst], writes=[BQ_], out=pq4[:, 1, tt, :], in0=iota_bf,
                         scalar1=tab3[:, 1, t:t + 1], scalar2=tab3[:, 2, t:t + 1], op0=ALU.is_equal, op1=ALU.mult)
                for q4 in range(4):
                    ps, Bp = gps2.next()
                    for t4 in range(4):
                        tt = q4 * 4 + t4
                        S.op("pe", "matmul", reads=list(pq_bufs[sb % 2][tt]), writes=[Bp], out=ps[:, t4 * 128:(t4 + 1) * 128], lhsT=pq4[:, 1, tt, :],
                             rhs=pq4[:, 0, tt, :], start=True, stop=True)
                    tg = sb * 16 + q4 * 4
                    S.op("act", "activation", reads=[Bp], writes=[BG], out=G3[:, :, tg:tg + 4],
                         in_=ps.rearrange("p (t i) -> p i t", t=4), func=AF.Copy)
            for c in range(128):
                ut, But = ut_ring.next()
                ut3 = ut.rearrange("p (k e) -> p k e", k=16)
                dma_in(ut, UTs[c], But, reads=[BUTs])
                ps, Bp = xps.next()
                for k in range(16):
                    S.op("pe", "matmul", reads=[But, Bn2d], writes=[Bp], out=ps[:, 0:TB], lhsT=ut3[:, k, :], rhs=n2d3[:, k, :],
                         start=(k == 0), stop=(k == 15))
                gt, Bgt = gt_ring.next()
                S.op("act", "activation", reads=[Bp], writes=[Bgt], out=gt, in_=ps[:, 0:TB], func=GELU)
                S.op("dve", "tensor_tensor", reads=[Bgt, BG], writes=[BG], out=G3[:, c, :], in0=gt, in1=G3[:, c, :], op=ALU.mult)
            hfs = []
            for tl in range(2):
                hf, Bhf = hf_ring.next()
                dma_in(hf, h1s[t0 + tl * 128:t0 + (tl + 1) * 128, :], Bhf, reads=[Bh1s[tb * 2 + tl]])
                hfs.append((hf, Bhf))
            for dh in range(2):
                for c in range(128):
                    vt, Bvt = vt_ring.next()
                    dma_in(vt, Vs[c * 128:(c + 1) * 128, dh * 1024:(dh + 1) * 1024], Bvt, reads=[BVs],
                           eng=("sp" if c % 2 == 0 else "act"))
                    for tl in range(2):
                        for nb in range(2):
                            b = 4 + tl * 2 + nb
                            S.op("pe", "matmul", reads=[BG, Bvt], writes=[Bps[b]], out=psum[b][:, :],
                                 lhsT=G3[:, c, tl * 128:(tl + 1) * 128], rhs=vt[:, nb * 512:(nb + 1) * 512],
                                 start=(c == 0), stop=(c == 127))
                for tl in range(2):
                    hf, Bhf = hfs[tl]
                    for nb in range(2):
                        b = 4 + tl * 2 + nb
                        sl = slice(dh * 1024 + nb * 512, dh * 1024 + (nb + 1) * 512)
                        S.op("dve", "tensor_tensor", reads=[Bps[b], Bhf], writes=[Bhf], out=hf[:, sl], in0=psum[b][:, :],
                             in1=hf[:, sl], op=ALU.add)
            for tl in range(2):
                hf, Bhf = hfs[tl]
                ot, Bot = hf, Bhf
                st, Bst = st_ring.next()
                S.op("act", "activation", reads=[Bhf], writes=[Bsqj, Bst], out=sqj, in_=hf, func=AF.Square, accum_out=st[:, 0:1])
                S.op("act", "activation", reads=[Bst], writes=[Bst], out=st[:, 1:2], in_=st[:, 0:1], func=AF.Sqrt, scale=1.0 / D,
                     bias=EPS)
                S.op("dve", "reciprocal", reads=[Bst], writes=[Bst], out=st[:, 2:3], in_=st[:, 1:2])
                S.op("act", "activation", reads=[Bhf, Bst], writes=[Bot], out=ot, in_=hf, func=AF.Copy, scale=st[:, 2:3])
                S.op("pool", "tensor_tensor", reads=[Bot, Bfg], writes=[Bot], out=ot, in0=ot, in1=fg, op=ALU.mult)
                S.op("pool", "dma_start", reads=[Bot], writes=[Bout], dma=Bot, out=out[t0 + tl * 128:t0 + (tl + 1) * 128, :], in_=ot)
        S.barrier()
        S.emit()
    return nc


def make_in_maps(inputs):
    x = np.asarray(inputs["x"], dtype=np.float32)
    sq = lambda k: np.ascontiguousarray(np.asarray(inputs[k], dtype=np.float32)[0])
    shared = {
        "mix_norm_g": sq("mix_norm_g"), "w_in": sq("w_in"), "conv_w": sq("conv_w"), "conv_b": sq("conv_b"),
        "lru_w_a": sq("lru_w_a"), "lru_b_a": sq("lru_b_a"), "lru_w_x": sq("lru_w_x"), "lru_b_x": sq("lru_b_x"),
        "lru_lambda": sq("lru_lambda"), "gmlp_ln_g": sq("gmlp_ln_g"), "gmlp_ln_b": sq("gmlp_ln_b"),
        "gmlp_w_s": sq("gmlp_w_s"), "gmlp_b_s": sq("gmlp_b_s"), "w_out": sq("w_out"), "ffn_norm_g": sq("ffn_norm_g"),
        "peer_w_q": sq("peer_w_q"), "peer_sub_keys": sq("peer_sub_keys"), "peer_u": sq("peer_u"), "peer_v": sq("peer_v"),
        "final_norm_g": np.ascontiguousarray(np.asarray(inputs["final_norm_g"], dtype=np.float32)),
    }
    in_maps = []
    for c in range(8):
        b, hf = c // 2, c % 2
        s0 = hf * T
        o0 = (1 - hf) * T

        def halo(st):
            h = np.zeros((128, D), np.float32)
            if st >= 2:
                h[0:2] = x[b, st - 2:st]
            if st + T < 2 * T:
                h[2] = x[b, st + T]
            return h
        d_o = 0 if hf == 1 else 1
        m = np.zeros((128, 2), np.float32)
        m[:, 0] = 1.0 if hf == 1 else 0.0
        m[:, 1] = 1.0 if hf == 0 else 0.0
        d = dict(shared)
        d.update({
            "x_own": np.ascontiguousarray(x[b, s0:s0 + T]), "x_halo": halo(s0),
            "x_oth": np.ascontiguousarray(x[b, o0:o0 + T]), "x_ohalo": halo(o0), "masks": m,
            "oth_w_a": np.ascontiguousarray(shared["lru_w_a"][d_o]), "oth_b_a": np.ascontiguousarray(shared["lru_b_a"][d_o]),
            "oth_w_x": np.ascontiguousarray(shared["lru_w_x"][d_o]), "oth_b_x": np.ascontiguousarray(shared["lru_b_x"][d_o]),
            "oth_lam": np.ascontiguousarray(shared["lru_lambda"][d_o]),
        })
        in_maps.append(d)
    return in_maps


_NC_CACHE = {}


def kernel(**inputs):
    if "nc" not in _NC_CACHE:
        _NC_CACHE["nc"] = build_nc()
    nc = _NC_CACHE["nc"]
    in_maps = make_in_maps(inputs)
    res = run_bass_kernel_spmd(nc, in_maps, core_ids=list(range(8)))
    outs = [np.asarray(res.results[c]["out"], dtype=np.float32) for c in range(8)]
    return np.stack(outs, 0).reshape(4, 2 * T, D)
```
